# Optimizing a Trainium2 kernel written in Bass

```python
import jax, jax.numpy as jnp
from jax import lax
import numpy as np

D_MODEL = 1024
BATCH = 32
SEQ = 2048
DEPTH = 2

N_A_LAYERS = DEPTH // 2
N_B_LAYERS = DEPTH - N_A_LAYERS
N_DENSE = (DEPTH + 1) // 2
N_MOE = DEPTH // 2
CONV_WIDTH = 31
N_HEADS = 8
HEAD_DIM = D_MODEL // N_HEADS
ROT_DIM = HEAD_DIM // 4
ROPE_THETA = 500000.0
MOBA_BLOCK = 256
MOBA_TOPK = 3
Q_CHUNK = 128
D_FF = ((8 * D_MODEL // 3 + 255) // 256) * 256
N_EXPERTS = 8
TOP_K = 2
D_FF_EXPERT = 7 * D_MODEL // 2
NORM_EPS = 1e-6
POS_OFFSET_MAX = 1024

kernel_name = 'hybrid_conformer_moba_yoco'


def _rms_norm(x, g):
    xf = x.astype(jnp.float32)
    y = xf * lax.rsqrt(jnp.mean(xf * xf, axis=-1, keepdims=True) + NORM_EPS)
    return (y * g.astype(jnp.float32)).astype(x.dtype)


def _layer_norm(x, g, b):
    xf = x.astype(jnp.float32)
    mu = jnp.mean(xf, axis=-1, keepdims=True)
    var = jnp.mean(jnp.square(xf - mu), axis=-1, keepdims=True)
    y = (xf - mu) * lax.rsqrt(var + NORM_EPS)
    return (y * g.astype(jnp.float32) + b.astype(jnp.float32)).astype(x.dtype)


def _ada(c, w, b, n):
    mod = jax.nn.silu(c) @ w + b
    return jnp.split(mod, n, axis=-1)


def _modulate(h, shift, scale):
    return h * (1.0 + scale[:, None, :]) + shift[:, None, :]


def _rotary(x, positions):
    half = ROT_DIM // 2
    inv_freq = ROPE_THETA ** (-jnp.arange(0, ROT_DIM, 2, dtype=jnp.float32) / ROT_DIM)
    ang = positions.astype(jnp.float32)[..., None] * inv_freq
    cos = jnp.cos(ang)[:, :, None, :]
    sin = jnp.sin(ang)[:, :, None, :]
    xr = x[..., :ROT_DIM].astype(jnp.float32)
    x1, x2 = xr[..., :half], xr[..., half:]
    rot = jnp.concatenate([x1 * cos - x2 * sin, x2 * cos + x1 * sin], axis=-1).astype(x.dtype)
    return jnp.concatenate([rot, x[..., ROT_DIM:]], axis=-1)


def _swiglu(h, w13, w2):
    a, g = jnp.split(h @ w13, 2, axis=-1)
    return (jax.nn.silu(a) * g) @ w2


def _conformer_conv(h, w1, b1, dw_w, dw_b, ln_g, ln_b, w2, b2):
    a, g = jnp.split(h @ w1 + b1, 2, axis=-1)
    u = a * jax.nn.sigmoid(g)
    z = lax.conv_general_dilated(u, dw_w[:, None, :], window_strides=(1,),
                                 padding=[(CONV_WIDTH - 1, 0)],
                                 dimension_numbers=('NWC', 'WIO', 'NWC'),
                                 feature_group_count=u.shape[-1]) + dw_b
    z = jax.nn.silu(_layer_norm(z, ln_g, ln_b))
    return z @ w2 + b2


def _shared_kv(x, c, positions, kv_ada_w, kv_ada_b, kv_norm_g, w_kv):
    b, s, _ = x.shape
    shift, scale = _ada(c, kv_ada_w, kv_ada_b, 2)
    h = _modulate(_rms_norm(x, kv_norm_g), shift, scale)
    k, v = jnp.split(h @ w_kv, 2, axis=-1)
    k = _rotary(k.reshape(b, s, N_HEADS, HEAD_DIM), positions)
    v = v.reshape(b, s, N_HEADS, HEAD_DIM)
    nb = -(-s // MOBA_BLOCK)
    pad = nb * MOBA_BLOCK - s
    k = jnp.pad(k, ((0, 0), (0, pad), (0, 0), (0, 0)))
    v = jnp.pad(v, ((0, 0), (0, pad), (0, 0), (0, 0)))
    kb = k.transpose(0, 2, 1, 3).reshape(b, N_HEADS, nb, MOBA_BLOCK, HEAD_DIM)
    vb = v.transpose(0, 2, 1, 3).reshape(b, N_HEADS, nb, MOBA_BLOCK, HEAD_DIM)
    kmean = jnp.mean(kb.astype(jnp.float32), axis=3).astype(kb.dtype)
    return kb, vb, kmean


def _moba_one_sequence(q, kb, vb, kmean):
    n_heads, seq, hd = q.shape
    nb = kb.shape[1]
    topk = min(MOBA_TOPK, max(nb - 1, 1))
    scale = hd ** -0.5
    head_idx = jnp.arange(n_heads)[:, None, None]
    blk_ids = jnp.arange(nb)

    def chunk(ci):
        t0 = ci * Q_CHUNK
        own = t0 // MOBA_BLOCK
        qc = lax.dynamic_slice_in_dim(q, t0, Q_CHUNK, axis=1)
        gate = jnp.einsum('hqd,hnd->hqn', qc, kmean).astype(jnp.float32)
        gate = jnp.where(blk_ids < own, gate, -jnp.inf)
        _, sel = lax.top_k(gate, topk)
        valid = sel < own
        k_sel = kb[head_idx, sel]
        v_sel = vb[head_idx, sel]
        s_sel = jnp.einsum('hqd,hqnbd->hqnb', qc, k_sel).astype(jnp.float32) * scale
        s_sel = jnp.where(valid[..., None], s_sel, -jnp.inf)
        k_own = lax.dynamic_index_in_dim(kb, own, axis=1, keepdims=False)
        v_own = lax.dynamic_index_in_dim(vb, own, axis=1, keepdims=False)
        s_own = jnp.einsum('hqd,hbd->hqb', qc, k_own).astype(jnp.float32) * scale
        q_pos = t0 + jnp.arange(Q_CHUNK)
        k_pos = own * MOBA_BLOCK + jnp.arange(MOBA_BLOCK)
        s_own = jnp.where(k_pos[None, None, :] <= q_pos[None, :, None], s_own, -jnp.inf)
        s = jnp.concatenate([s_sel.reshape(n_heads, Q_CHUNK, topk * MOBA_BLOCK), s_own], axis=-1)
        p = jax.nn.softmax(s, axis=-1).astype(vb.dtype)
        p_sel = p[..., :topk * MOBA_BLOCK].reshape(n_heads, Q_CHUNK, topk, MOBA_BLOCK)
        p_own = p[..., topk * MOBA_BLOCK:]
        return (jnp.einsum('hqnb,hqnbd->hqd', p_sel, v_sel)
                + jnp.einsum('hqb,hbd->hqd', p_own, v_own))

    out = lax.map(chunk, jnp.arange(seq // Q_CHUNK))
    return out.transpose(1, 0, 2, 3).reshape(n_heads, seq, hd)


def _moba_attention(q, kb, vb, kmean):
    return lax.map(lambda args: _moba_one_sequence(*args), (q, kb, vb, kmean))


def _moe_ffn(h, router_w, router_b, w13, w2):
    b, s, d = h.shape
    t = h.reshape(-1, d)
    logits = t.astype(jnp.float32) @ router_w.astype(jnp.float32) + router_b.astype(jnp.float32)
    top_val, top_idx = lax.top_k(logits, TOP_K)
    top_w = jax.nn.softmax(top_val, axis=-1)
    gates = jnp.sum(jax.nn.one_hot(top_idx, N_EXPERTS, dtype=jnp.float32) * top_w[..., None], axis=1)
    y = jnp.zeros(t.shape, jnp.float32)
    for e in range(N_EXPERTS):
        y = y + gates[:, e:e + 1] * _swiglu(t, w13[e], w2[e]).astype(jnp.float32)
    return y.astype(h.dtype).reshape(b, s, d)


def setup_inputs(seed: int = 0) -> dict:
    key = jax.random.key(seed)
    ks = iter(jax.random.split(key, 32))
    D = D_MODEL
    f32 = jnp.float32

    def nrm(shape, fan_in, mult=1.0):
        return jax.random.normal(next(ks), shape, f32) * (mult * fan_in ** -0.5)

    def small(shape, s=0.02):
        return jax.random.normal(next(ks), shape, f32) * s

    def gain(shape):
        return 1.0 + small(shape)

    x = jax.random.normal(next(ks), (BATCH, SEQ, D), f32)
    c = jax.random.normal(next(ks), (BATCH, D), f32)
    positions = (jnp.arange(SEQ, dtype=jnp.int32)[None, :]
                 + jax.random.randint(next(ks), (BATCH, 1), 0, POS_OFFSET_MAX, dtype=jnp.int32))
    return {
        'x': x, 'c': c, 'positions': positions,
        'ada_w': nrm((DEPTH, D, 6 * D), D, 0.5), 'ada_b': small((DEPTH, 6 * D)),
        'norm1_g': gain((DEPTH, D)), 'norm2_g': gain((DEPTH, D)),
        'conv_w1': nrm((N_A_LAYERS, D, 2 * D), D), 'conv_b1': small((N_A_LAYERS, 2 * D)),
        'conv_dw_w': nrm((N_A_LAYERS, CONV_WIDTH, D), CONV_WIDTH), 'conv_dw_b': small((N_A_LAYERS, D)),
        'conv_ln_g': gain((N_A_LAYERS, D)), 'conv_ln_b': small((N_A_LAYERS, D)),
        'conv_w2': nrm((N_A_LAYERS, D, D), D), 'conv_b2': small((N_A_LAYERS, D)),
        'kv_ada_w': nrm((D, 2 * D), D, 0.5), 'kv_ada_b': small((2 * D,)),
        'kv_norm_g': gain((D,)), 'w_kv': nrm((D, 2 * D), D),
        'w_q': nrm((N_B_LAYERS, D, D), D), 'w_o': nrm((N_B_LAYERS, D, D), D),
        'ffn_w13': nrm((N_DENSE, D, 2 * D_FF), D), 'ffn_w2': nrm((N_DENSE, D_FF, D), D_FF),
        'router_w': nrm((N_MOE, D, N_EXPERTS), D), 'router_b': small((N_MOE, N_EXPERTS), 0.01),
        'moe_w13': nrm((N_MOE, N_EXPERTS, D, 2 * D_FF_EXPERT), D),
        'moe_w2': nrm((N_MOE, N_EXPERTS, D_FF_EXPERT, D), D_FF_EXPERT),
        'final_g': gain((D,)),
    }


def reference(x, c, positions, ada_w, ada_b, norm1_g, norm2_g, conv_w1, conv_b1, conv_dw_w,
              conv_dw_b, conv_ln_g, conv_ln_b, conv_w2, conv_b2, kv_ada_w, kv_ada_b, kv_norm_g,
              w_kv, w_q, w_o, ffn_w13, ffn_w2, router_w, router_b, moe_w13, moe_w2, final_g):
    b, s, d = x.shape
    kb = vb = kmean = None
    for i in range(DEPTH):
        shift1, scale1, gate1, shift2, scale2, gate2 = _ada(c, ada_w[i], ada_b[i], 6)
        h = _modulate(_rms_norm(x, norm1_g[i]), shift1, scale1)
        if i < N_A_LAYERS:
            mix = _conformer_conv(h, conv_w1[i], conv_b1[i], conv_dw_w[i], conv_dw_b[i],
                                  conv_ln_g[i], conv_ln_b[i], conv_w2[i], conv_b2[i])
        else:
            j = i - N_A_LAYERS
            q = _rotary((h @ w_q[j]).reshape(b, s, N_HEADS, HEAD_DIM), positions)
            o = _moba_attention(q.transpose(0, 2, 1, 3), kb, vb, kmean)
            mix = o.transpose(0, 2, 1, 3).reshape(b, s, d) @ w_o[j]
        x = x + gate1[:, None, :] * mix
        h = _modulate(_rms_norm(x, norm2_g[i]), shift2, scale2)
        if i % 2 == 0:
            f = _swiglu(h, ffn_w13[i // 2], ffn_w2[i // 2])
        else:
            f = _moe_ffn(h, router_w[i // 2], router_b[i // 2], moe_w13[i // 2], moe_w2[i // 2])
        x = x + gate2[:, None, :] * f
        if i == N_A_LAYERS - 1:
            kb, vb, kmean = _shared_kv(x, c, positions, kv_ada_w, kv_ada_b, kv_norm_g, w_kv)
    return _rms_norm(x, final_g)
```

```python
import math
from contextlib import ExitStack

import numpy as np
import concourse.bass as bass
import concourse.mybir as mybir
from concourse.bass_utils import run_bass_kernel_spmd

F32 = mybir.dt.float32
BF16 = mybir.dt.bfloat16
I32 = mybir.dt.int32
AF = mybir.ActivationFunctionType
ALU = mybir.AluOpType
AX = mybir.AxisListType

D = 1024
S = 2048
KC = 8
NH = 8
TT = 512
NT = S // TT
FF0 = 2816
FFE = 3584
NE = 8
CW = 31
EPS = 1e-6
BIG = 30000.0
ATT_SCALE = 128 ** -0.5
N_CORES = 8
ATT_DEPTH = 2
MOE_NCH = FFE // 128
MOE_SEG = 7
MOE_NSEG = MOE_NCH // MOE_SEG

ENGS = ("pe", "act", "dve", "pool", "sp")


class Buf:
    __slots__ = ("name", "w", "r")

    def __init__(self, name=""):
        self.name = name
        self.w = None
        self.r = []


class Op:
    __slots__ = ("eng", "fn", "deps", "signal", "count", "dma", "slot", "val")

    def __init__(self, eng, fn, deps, dma=False):
        self.eng = eng
        self.fn = fn
        self.deps = deps
        self.signal = False
        self.count = 0
        self.dma = dma
        self.slot = None
        self.val = 0


class Prog:
    NSLOT = 8

    def __init__(self, nc, es):
        self.nc = nc
        self.ops = {e: [] for e in ENGS}
        self.sem = {e: es.enter_context(nc.semaphore("sem_" + e)) for e in ENGS}
        self.dq = {q: [es.enter_context(nc.semaphore("dq_%s%d" % (q, i))) for i in range(self.NSLOT)] for q in ("sp", "pool")}
        self.ndma = {"sp": 0, "pool": 0}

    @staticmethod
    def _deps(r, w):
        deps = []
        for b in r:
            if b.w is not None:
                deps.append(b.w)
        for b in w:
            if b.w is not None:
                deps.append(b.w)
            deps.extend(b.r)
        return deps

    @staticmethod
    def _commit(op, r, w):
        for b in r:
            b.r.append(op)
        for b in w:
            b.w = op
            b.r = []

    def op(self, eng, fn, r=(), w=()):
        o = Op(eng, fn, self._deps(r, w))
        self.ops[eng].append(o)
        self._commit(o, r, w)
        return o

    def dma(self, out, in_, r=(), w=(), q="sp"):
        o = Op(q, (out, in_), self._deps(r, w), dma=True)
        i = self.ndma[q]
        self.ndma[q] += 1
        o.slot = i % self.NSLOT
        o.val = 16 * (i // self.NSLOT + 1)
        self.ops[q].append(o)
        self._commit(o, r, w)
        return o

    def emit(self):
        nc = self.nc
        for e in ENGS:
            for o in self.ops[e]:
                for d in o.deps:
                    if not d.dma:
                        d.signal = True
        for e in ENGS:
            c = 0
            for o in self.ops[e]:
                if not o.dma and o.signal:
                    c += 1
                    o.count = c

        def run(e, h):
            seen = {}
            for o in self.ops[e]:
                waits = {}
                for d in o.deps:
                    if d.dma:
                        key = ("d", d.eng, d.slot)
                        v = d.val
                    else:
                        key = ("e", d.eng)
                        v = d.count
                    if v > waits.get(key, 0):
                        waits[key] = v
                if o.dma and o.val > 16:
                    key = ("d", o.eng, o.slot)
                    waits[key] = max(waits.get(key, 0), o.val - 16)
                for key, v in waits.items():
                    if seen.get(key, 0) >= v:
                        continue
                    seen[key] = v
                    s = self.dq[key[1]][key[2]] if key[0] == "d" else self.sem[key[1]]
                    h.wait_ge(s, v)
                if o.dma:
                    out, in_ = o.fn
                    h.dma_start(out=out, in_=in_).then_inc(self.dq[o.eng][o.slot], 16)
                else:
                    ins = o.fn(h)
                    if o.signal:
                        ins.then_inc(self.sem[e], 1)

        with nc.Block() as block:
            @block.tensor
            def _(h):
                run("pe", h)

            @block.scalar
            def _(h):
                run("act", h)

            @block.vector
            def _(h):
                run("dve", h)

            @block.gpsimd
            def _(h):
                run("pool", h)

            @block.sync
            def _(h):
                run("sp", h)


class Ring:
    def __init__(self, t, n, name):
        self.t = t
        self.n = n
        self.b = [Buf("%s%d" % (name, i)) for i in range(n)]
        self.i = 0

    def next(self):
        i = self.i % self.n
        self.i += 1
        return self.t[:, i], self.b[i]


def _cols(v):
    v = np.asarray(v, np.float32).reshape(-1, 128)
    return np.ascontiguousarray(v.T)


VEC_LAYOUT = {}


def _vec_layout():
    if VEC_LAYOUT:
        return VEC_LAYOUT
    off = 0
    for name, n in (("ada_b0", 48), ("ada_b1", 48), ("kv_ada_b", 16), ("n1g0", 8), ("n1g1", 8),
                    ("n2g0", 8), ("n2g1", 8), ("kvg", 8), ("fing", 8), ("cb1", 16), ("dwb", 8),
                    ("lng", 8), ("lnb", 8), ("cb2", 8), ("dww", 8 * CW)):
        VEC_LAYOUT[name] = (off, n)
        off += n
    VEC_LAYOUT["_n"] = (off, 0)
    return VEC_LAYOUT


CONST_LAYOUT = {}


def _const_layout():
    if CONST_LAYOUT:
        return CONST_LAYOUT
    off = 0
    for name, n in (("ident", 128), ("smask", 64), ("invf", 1), ("sgn", 1), ("pm", 32)):
        CONST_LAYOUT[name] = (off, n)
        off += n
    CONST_LAYOUT["_n"] = (off, 0)
    return CONST_LAYOUT


NC2 = 12 * 128


def _make_consts():
    L = _const_layout()
    c = np.zeros((128, L["_n"][0]), np.float32)
    o = L["ident"][0]
    c[:, o:o + 128] = np.eye(128, dtype=np.float32)
    o = L["smask"][0]
    for b in range(8):
        for n in range(8):
            c[:, o + b * 8 + n] = -BIG if n >= b else 0.0
    o = L["invf"][0]
    i = np.arange(32)
    invf = 500000.0 ** (-(2.0 * (i % 16)) / 32.0)
    c[:32, o] = (invf / (2.0 * math.pi)).astype(np.float32)
    o = L["sgn"][0]
    c[:16, o] = -1.0
    c[16:32, o] = 1.0
    o = L["pm"][0]
    for col in range(32):
        partner = col + 16 if col < 16 else col - 16
        c[partner, o + col] = 1.0
    c2 = np.zeros((128, NC2), np.float32)
    c2[:, 0:128] = np.eye(128, dtype=np.float32)
    c2[:, 128:256] = 1.0
    kk = np.arange(128)[:, None]
    qq = np.arange(128)[None, :]
    c2[:, 256:384] = (kk <= qq).astype(np.float32)
    o = 384
    for n in range(8):
        for r in (n, 8, 9):
            c2[r, o + n * 128:o + (n + 1) * 128] = 1.0
    c2[8, o + 8 * 128:o + 9 * 128] = 1.0
    return c, c2


def build(nseq, dbg=None):
    nc = bass.Bass("TRN2", target_bir_lowering=False)
    VL = _vec_layout()
    CL = _const_layout()
    NTOK = nseq * S
    NVEC = VL["_n"][0]
    NCON = CL["_n"][0]

    def din(name, shape, dt=F32):
        return nc.dram_tensor(name, shape, dt, kind="ExternalInput").ap()

    x_d = din("x", [NTOK, D])
    cT_d = din("cT", [128, KC * nseq])
    pos_d = din("pos", [nseq, S], I32)
    vec_d = din("vecs", [128, NVEC])
    con_d = din("consts", [128, NCON])
    con2_d = din("consts2", [128, NC2])
    ada_w_d = din("ada_w", [2 * D, 6 * D])
    kv_ada_w_d = din("kv_ada_w", [D, 2 * D])
    conv_w1_d = din("conv_w1", [D, 2 * D])
    conv_w2_d = din("conv_w2", [D, D])
    w_kv_d = din("w_kv", [D, 2 * D])
    w_q_d = din("w_q", [D, D])
    w_o_d = din("w_o", [D, D])
    ffn_w13_d = din("ffn_w13", [D, 2 * FF0])
    ffn_w2_d = din("ffn_w2", [FF0, D])
    router_w_d = din("router_w", [D, NE])
    router_b_d = din("router_b", [1, NE])
    moe_w13_d = din("moe_w13", [NE * D, 2 * FFE])
    moe_w2_d = din("moe_w2", [NE * FFE, D])
    if dbg is None:
        out_d = nc.dram_tensor("out", [NTOK, D], F32, kind="ExternalOutput").ap()
    else:
        out_d = nc.dram_tensor("out", list(dbg[1]), F32, kind="ExternalOutput").ap()
    k_scr = nc.dram_tensor("k_scr", [NH * 128, S], BF16, kind="Internal").ap()
    v_scr = nc.dram_tensor("v_scr", [S, D], BF16, kind="Internal").ap()
    kscr_b = [[Buf("kscr%d_%d" % (h_, t_)) for t_ in range(NT)] for h_ in range(NH)]
    vscr_b = [Buf("vscr%d" % t_) for t_ in range(S // 128)]
    moe_scr13 = nc.dram_tensor("moe_scr13", [NE * MOE_NCH * 128, 2 * KC * 128], BF16, kind="Internal").ap()
    moe_scr2 = nc.dram_tensor("moe_scr2", [NE * MOE_NSEG * KC * 128, MOE_SEG * 128], BF16, kind="Internal").ap()
    scr13_b = [[Buf("s13_%d_%d" % (e, c)) for c in range(MOE_NCH)] for e in range(NE)]
    scr2_b = [[[Buf("s2_%d_%d_%d" % (e, sg, j)) for j in range(KC)] for sg in range(MOE_NSEG)] for e in range(NE)]
    out_bufs = []

    es = ExitStack()
    with es:
        P = Prog(nc, es)

        def sb(name, shape, dt=F32):
            return es.enter_context(nc.sbuf_tensor(name, shape, dt))

        def ps(name, shape, dt=F32):
            return es.enter_context(nc.psum_tensor(name, shape, dt))

        xT = sb("xT", [128, KC, S])
        xb = [[Buf("x%d_%d" % (k, t)) for t in range(NT)] for k in range(KC)]
        A = sb("A", [128, KC, S], BF16)
        Ab = [Buf("A%d" % t) for t in range(NT)]
        G32 = sb("G", [128, 12288])
        G = G32[:].bitcast(BF16)
        Gb = Buf("G")
        WS = 2048
        wst_t = sb("wst", [128, 2, WS])
        wst = Ring(wst_t, 2, "wst")
        wbf_t = sb("wbf", [128, 3, WS], BF16)
        wbf = Ring(wbf_t, 3, "wbf")
        tmp_t = sb("tmpf", [128, 4, TT])
        tmp = Ring(tmp_t, 4, "tmp")
        vecs = sb("vecs_sb", [128, NVEC])
        vecs_b = Buf("vecs")
        con = sb("con_sb", [128, NCON])
        con_b = Buf("con")
        cbf = sb("cbf", [128, NC2], BF16)
        cbf_b = Buf("cbf")
        modv = sb("modv", [128, 112, nseq])
        modv_b = Buf("modv")
        gsv = sb("gsv", [128, 5, KC, nseq])
        gsv_b = Buf("gsv")
        sc = sb("sc", [128, KC * nseq])
        sc_b = Buf("sc")
        small = sb("small", [128, 64])
        ones_f = sb("ones_f", [128, 128])
        ones_fb = Buf("ones_f")
        rot_t = sb("rot", [32, 2, S], BF16)
        rot_b = Buf("rot")
        kmb = sb("kmb", [128, NH, 8], BF16)
        kmb_b = Buf("kmb")
        nhk = sb("nhk", [128, NH])
        nhk_b = Buf("nhk")
        gates = sb("gates", [128, S // 128, NE])
        gates_b = Buf("gates")
        rall = sb("rall", [10, S], BF16)
        rall_b = Buf("rall")
        rt_t = sb("rt", [128, 4, 16])
        rt = Ring(rt_t, 4, "rt")
        pt_t = sb("pt", [128, 4, 256], BF16)
        pt = Ring(pt_t, 4, "pt")
        rwf = sb("rwf", [128, KC, NE])
        rbf = sb("rbf", [1, NE])
        rw_b = Buf("rw")

        pA_t = ps("pA", [128, 4, TT])
        pA = Ring(pA_t, 4, "pA")
        pB_t = ps("pB", [128, 2, TT])
        pB = Ring(pB_t, 2, "pB")
        pC_t = ps("pC", [128, 2, TT])
        pC_b = Buf("pC")
        pC0b, pC1b = Buf("pC0"), Buf("pC1")

        ident_f = con[:, CL["ident"][0]:CL["ident"][0] + 128]
        ident_bf = cbf[:, 0:128]
        ones_bf = cbf[:, 128:256]
        tri_bf = cbf[:, 256:384]
        esel_bf = cbf[:, 384:384 + 9 * 128]

        def vcol(name, i):
            o = VL[name][0] + i
            return vecs[:, o:o + 1]

        def ccol(name, i=0, n=1, rows=128):
            o = CL[name][0] + i
            return con[0:rows, o:o + n]

        def chain(eng, fns, r=(), w=()):
            for f in fns:
                P.op(eng, f, r=r, w=w)

        P.dma(vecs[:], vec_d[:, :], w=[vecs_b])
        P.dma(con[:], con_d[:, :], w=[con_b])
        P.dma(sc[:], cT_d[:, :], w=[sc_b])
        P.dma(rwf[:], router_w_d.rearrange("(k p) n -> p k n", p=128), w=[rw_b])
        P.dma(rbf[:], router_b_d[:, :], w=[rw_b])
        P.op("pool", lambda h: h.memset(ones_f[:], 1.0), w=[ones_fb])
        st0, st0b = wst.next()
        P.dma(st0[:, 0:NC2], con2_d[:, :], w=[st0b])
        P.op("dve", lambda h: h.tensor_copy(out=cbf[:], in_=st0[:, 0:NC2]), r=[st0b], w=[cbf_b])
        P.op("act", lambda h: h.activation(out=sc[:], in_=sc[:], func=AF.Silu), r=[sc_b], w=[sc_b])

        cast_rr = [0]

        def load_w(pieces, ncols):
            wb, wbb = wbf.next()
            for (co, ap, kc, n) in pieces:
                st, stb = wst.next()
                P.dma(st[:, 0:kc * n].rearrange("p (k n) -> p k n", n=n), ap, w=[stb])
                P.op("pool", lambda h, st=st, co=co, kc=kc, n=n: h.tensor_copy(out=wb[:, co:co + kc * n], in_=st[:, 0:kc * n]),
                     r=[stb], w=[wbb])
            return wb, wbb

        def mm_group(out_ap, pairs, r, w):
            def fn(h):
                n = len(pairs)
                ins = None
                for i, (l, rr) in enumerate(pairs):
                    ins = h.matmul(out_ap, lhsT=l, rhs=rr, start=(i == 0), stop=(i == n - 1))
                return ins
            return P.op("pe", fn, r=r, w=w)

        def ada(w_ap, nchunks, bias_name, dst_off):
            wv = w_ap.rearrange("(k p) n -> p k n", p=128)
            for blk in range(nchunks // 2):
                st, stb = wst.next()
                stv = st[:, 0:KC * 256].rearrange("p (k n) -> p k n", n=256)
                P.dma(stv, wv[:, :, blk * 256:(blk + 1) * 256], w=[stb])
                for m in range(2):
                    idx = blk * 2 + m
                    pt_, pb_ = pA.next()
                    mm_group(pt_[:, 0:nseq],
                             [(stv[:, k, m * 128:(m + 1) * 128], sc[:, k * nseq:(k + 1) * nseq]) for k in range(KC)],
                             r=[stb, sc_b], w=[pb_])
                    P.op("act", lambda h, pt_=pt_, idx=idx: h.activation(
                        out=modv[:, dst_off + idx, :], in_=pt_[:, 0:nseq], func=AF.Identity,
                        bias=vcol(bias_name, idx), scale=1.0), r=[pb_, vecs_b], w=[modv_b])

        ada(ada_w_d[0:D, :], 48, "ada_b0", 0)
        ada(ada_w_d[D:2 * D, :], 48, "ada_b1", 48)
        ada(kv_ada_w_d, 16, "kv_ada_b", 96)
        GS_SRC = [(8, "n1g0"), (32, "n2g0"), (96 + 8, "kvg"), (48 + 8, "n1g1"), (48 + 32, "n2g1")]
        SHIFT_SRC = [0, 24, 96, 48, 48 + 24]
        for wi, (so, gname) in enumerate(GS_SRC):
            go = VL[gname][0]
            for s_ in range(nseq):
                P.op("dve", lambda h, wi=wi, so=so, go=go, s_=s_: h.scalar_tensor_tensor(
                    out=gsv[:, wi, :, s_], in0=modv[:, so:so + 8, s_], scalar=1.0,
                    in1=vecs[:, go:go + 8], op0=ALU.add, op1=ALU.mult), r=[modv_b, vecs_b], w=[gsv_b])

        sqv = G[:, 0:KC * TT].rearrange("p (k n) -> p k n", n=TT)
        hfv = G32[:, KC * TT // 2:KC * TT // 2 + KC * TT].rearrange("p (k n) -> p k n", n=TT)
        hfb = Buf("hf")

        def rmsnorm(s_, which, tiles, f32=False):
            for t in tiles:
                tsl = slice(t * TT, (t + 1) * TT)
                P.op("pool", lambda h, tsl=tsl: h.tensor_tensor(out=sqv, in0=xT[:, :, tsl], in1=xT[:, :, tsl], op=ALU.mult),
                     r=[xb[k][t] for k in range(KC)], w=[Gb])
                pss, pssb = pB.next()
                mm_group(pss, [(ones_bf, sqv[:, k, :]) for k in range(KC)], r=[Gb, cbf_b], w=[pssb])
                sd, sdb = pss, pssb
                P.op("act", lambda h, sd=sd, pss=pss: h.activation(out=sd, in_=pss, func=AF.Sqrt, bias=EPS, scale=1.0 / D),
                     r=[pssb], w=[sdb])
                P.op("dve", lambda h, sd=sd: h.reciprocal(out=sd, in_=sd), r=[sdb], w=[sdb])
                for k in range(KC):
                    if which is None:
                        P.op("dve", lambda h, k=k, sd=sd, tsl=tsl: h.scalar_tensor_tensor(
                            out=hfv[:, k, :], in0=xT[:, k, tsl], scalar=vcol("fing", k), in1=sd,
                            op0=ALU.mult, op1=ALU.mult), r=[xb[k][t], sdb, vecs_b], w=[hfb])
                        continue
                    t1, t1b = tmp.next()
                    P.op("dve", lambda h, k=k, sd=sd, t1=t1, tsl=tsl: h.scalar_tensor_tensor(
                        out=t1, in0=xT[:, k, tsl], scalar=gsv[:, which, k, s_:s_ + 1], in1=sd,
                        op0=ALU.mult, op1=ALU.mult), r=[xb[k][t], sdb, gsv_b], w=[t1b])
                    sh = modv[:, SHIFT_SRC[which] + k, s_:s_ + 1]
                    if f32:
                        P.op("act", lambda h, k=k, t1=t1, sh=sh: h.activation(
                            out=hfv[:, k, :], in_=t1, func=AF.Identity, bias=sh, scale=1.0),
                            r=[t1b, modv_b], w=[hfb])
                        P.op("pool", lambda h, k=k, tsl=tsl: h.tensor_copy(out=A[:, k, tsl], in_=hfv[:, k, :]),
                             r=[hfb], w=[Ab[t]])
                    else:
                        P.op("act", lambda h, k=k, t1=t1, sh=sh, tsl=tsl: h.activation(
                            out=A[:, k, tsl], in_=t1, func=AF.Identity, bias=sh, scale=1.0),
                            r=[t1b, modv_b], w=[Ab[t]])

        def ffn(load13, load2, ff, seg, tiles, epilogue, mid_hook=None):
            nch = ff // 128
            ntl = len(tiles)
            Gv = G[:, 0:seg * ntl * TT].rearrange("p (c n) -> p c n", n=ntl * TT)
            for c0 in range(0, nch, seg):
                for cc in range(seg):
                    c = c0 + cc
                    wb, wbb = load13(c)
                    wa = wb[:, 0:KC * 128].rearrange("p (k n) -> p k n", n=128)
                    wg = wb[:, KC * 128:2 * KC * 128].rearrange("p (k n) -> p k n", n=128)
                    for ti, t in enumerate(tiles):
                        tsl = slice(t * TT, (t + 1) * TT)
                        pa, pab = pA.next()
                        pg, pgb = pA.next()

                        def fn_ag(h, wa=wa, wg=wg, pa=pa, pg=pg, tsl=tsl):
                            ins = None
                            for k in range(KC):
                                ins = h.matmul(pa, lhsT=wa[:, k, :], rhs=A[:, k, tsl], start=(k == 0), stop=(k == KC - 1))
                            for k in range(KC):
                                ins = h.matmul(pg, lhsT=wg[:, k, :], rhs=A[:, k, tsl], start=(k == 0), stop=(k == KC - 1))
                            return ins
                        P.op("pe", fn_ag, r=[wbb, Ab[t]], w=[pab, pgb])
                        sa, sab = tmp.next()
                        P.op("act", lambda h, sa=sa, pa=pa: h.activation(out=sa, in_=pa, func=AF.Silu), r=[pab], w=[sab])
                        P.op("dve", lambda h, sa=sa, pg=pg, cc=cc, ti=ti: h.tensor_tensor(
                            out=Gv[:, cc, ti * TT:(ti + 1) * TT], in0=pg, in1=sa, op=ALU.mult), r=[sab, pgb], w=[Gb])
                if mid_hook is not None and c0 == 0:
                    mid_hook()
                for j in range(KC):
                    wb, wbb = load2(c0, seg, j)
                    w2b = wb[:, 0:seg * 128].rearrange("p (c n) -> p c n", n=128)
                    for ti, t in enumerate(tiles):
                        py, pyb = pA.next()
                        mm_group(py, [(w2b[:, cc, :], Gv[:, cc, ti * TT:(ti + 1) * TT]) for cc in range(seg)],
                                 r=[wbb, Gb], w=[pyb])
                        epilogue(j, t, ti, py, pyb)

        def f32_loaders(w13_ap, w2_ap, ff):
            w13v = w13_ap.rearrange("(k p) n -> p k n", p=128)
            w2v = w2_ap.rearrange("(c p) n -> p c n", p=128)

            def l13(c):
                return load_w([(0, w13v[:, :, c * 128:(c + 1) * 128], KC, 128),
                               (KC * 128, w13v[:, :, ff + c * 128:ff + (c + 1) * 128], KC, 128)], 2 * KC * 128)

            def l2(c0, seg, j):
                return load_w([(0, w2v[:, c0:c0 + seg, j * 128:(j + 1) * 128], seg, 128)], seg * 128)
            return l13, l2

        def resid_epilogue(gate_off, s_):
            def ep(j, t, ti, py, pyb):
                tsl = slice(t * TT, (t + 1) * TT)
                P.op("dve", lambda h: h.scalar_tensor_tensor(
                    out=xT[:, j, tsl], in0=py, scalar=modv[:, gate_off + j, s_:s_ + 1], in1=xT[:, j, tsl],
                    op0=ALU.mult, op1=ALU.add), r=[pyb, modv_b, xb[j][t]], w=[xb[j][t]])
            return ep

        def rotary_tables(s_):
            for t in range(NT):
                tsl = slice(t * TT, (t + 1) * TT)
                ya, yab = tmp.next()
                yb, ybb = tmp.next()
                yc, ycb = tmp.next()
                ya, yb, yc = ya[0:32, :], yb[0:32, :], yc[0:32, :]
                yai = ya.bitcast(I32)
                P.dma(yai, pos_d[s_:s_ + 1, tsl].to_broadcast([32, TT]), w=[yab])
                fns = [lambda h, yb=yb, yai=yai: h.tensor_copy(out=yb, in_=yai),
                       lambda h, yb=yb: h.tensor_scalar(out=yb, in0=yb, scalar1=ccol("invf", rows=32), scalar2=None, op0=ALU.mult)]
                for which, add in ((1, 0.0), (0, 0.25)):
                    fns += [
                        lambda h, yc=yc, yb=yb, add=add: h.tensor_scalar(out=yc, in0=yb, scalar1=add, scalar2=None, op0=ALU.add),
                        lambda h, yc=yc, yai=yai: h.tensor_copy(out=yai, in_=yc),
                        lambda h, ya=ya, yai=yai: h.tensor_copy(out=ya, in_=yai),
                        lambda h, yc=yc, ya=ya: h.tensor_tensor(out=yc, in0=yc, in1=ya, op=ALU.subtract),
                        lambda h, yc=yc, ya=ya: h.tensor_scalar(out=ya, in0=yc, scalar1=0.5, scalar2=None, op0=ALU.is_gt),
                        lambda h, yc=yc, ya=ya: h.tensor_tensor(out=yc, in0=yc, in1=ya, op=ALU.subtract),
                        lambda h, yc=yc, ya=ya: h.tensor_scalar(out=ya, in0=yc, scalar1=-0.5, scalar2=None, op0=ALU.is_lt),
                        lambda h, yc=yc, ya=ya: h.tensor_tensor(out=yc, in0=yc, in1=ya, op=ALU.add),
                    ]
                    chain("dve", fns, r=[con_b], w=[yab, ybb, ycb])
                    fns = []
                    P.op("act", lambda h, yc=yc: h.activation(out=yc, in_=yc, func=AF.Sin, scale=2.0 * math.pi * (1.0 - 1e-6)),
                         w=[ycb])
                    if which == 1:
                        P.op("dve", lambda h, yc=yc, tsl=tsl: h.tensor_scalar(
                            out=rot_t[:, 1, tsl], in0=yc, scalar1=ccol("sgn", rows=32), scalar2=None, op0=ALU.mult),
                            r=[ycb, con_b], w=[rot_b])
                    else:
                        P.op("dve", lambda h, yc=yc, tsl=tsl: h.tensor_copy(out=rot_t[:, 0, tsl], in_=yc), r=[ycb], w=[rot_b])

        def apply_rotary(kf, kfb, t):
            tsl = slice(t * TT, (t + 1) * TT)
            pr, prb = pB.next()
            pm = ccol("pm", 0, 32, rows=32)
            P.op("pe", lambda h: h.matmul(pr[0:32, :], lhsT=pm, rhs=kf[0:32, :], start=True, stop=True),
                 r=[kfb, con_b], w=[prb])
            t2, t2b = tmp.next()
            chain("dve", [
                lambda h: h.tensor_tensor(out=t2[0:32, :], in0=pr[0:32, :], in1=rot_t[:, 1, tsl], op=ALU.mult),
                lambda h: h.tensor_tensor(out=kf[0:32, :], in0=kf[0:32, :], in1=rot_t[:, 0, tsl], op=ALU.mult),
                lambda h: h.tensor_tensor(out=kf[0:32, :], in0=kf[0:32, :], in1=t2[0:32, :], op=ALU.add),
            ], r=[prb, rot_b], w=[kfb, t2b])

        def dbg_dump_xT():
            for k in range(KC):
                b_ = Buf()
                P.dma(out_d[:, k * S:(k + 1) * S], xT[:, k, :], r=[xb[k][t] for t in range(NT)], w=[b_])
                out_bufs.append(b_)

        pre = Ring(G32[:, 0:12288].rearrange("p (s n) -> p s n", n=1024), 12, "pre")
        cast_i = [0]

        def pre_cast(dst, src, srcb, dstb):
            eng = ("dve", "act")[cast_i[0] % 2]
            cast_i[0] += 1
            if eng == "dve":
                P.op("dve", lambda h: h.tensor_copy(out=dst, in_=src), r=[srcb], w=[dstb])
            else:
                P.op("act", lambda h: h.copy(out=dst, in_=src), r=[srcb], w=[dstb])

        if dbg is None or dbg[0] in ("x4",):
            for e in range(NE):
                w13v = moe_w13_d[e * D:(e + 1) * D, :].rearrange("(k p) n -> p k n", p=128)
                w2v = moe_w2_d[e * FFE:(e + 1) * FFE, :].rearrange("(c p) n -> p c n", p=128)
                for c in range(MOE_NCH):
                    wb, wbb = wbf.next()
                    for half, co in ((0, c * 128), (1, FFE + c * 128)):
                        st, stb = pre.next()
                        P.dma(st[:, 0:KC * 128].rearrange("p (k n) -> p k n", n=128), w13v[:, :, co:co + 128], w=[stb])
                        pre_cast(wb[:, half * KC * 128:(half + 1) * KC * 128], st[:, 0:KC * 128], stb, wbb)
                    r0 = (e * MOE_NCH + c) * 128
                    P.dma(moe_scr13[r0:r0 + 128, :], wb[:, 0:2 * KC * 128], r=[wbb], w=[scr13_b[e][c]], q="pool")
                for sg in range(MOE_NSEG):
                    for j in range(KC):
                        wb, wbb = wbf.next()
                        st, stb = pre.next()
                        P.dma(st[:, 0:MOE_SEG * 128].rearrange("p (c n) -> p c n", n=128),
                              w2v[:, sg * MOE_SEG:(sg + 1) * MOE_SEG, j * 128:(j + 1) * 128], w=[stb])
                        pre_cast(wb[:, 0:MOE_SEG * 128], st[:, 0:MOE_SEG * 128], stb, wbb)
                        r0 = ((e * MOE_NSEG + sg) * KC + j) * 128
                        P.dma(moe_scr2[r0:r0 + 128, :], wb[:, 0:MOE_SEG * 128], r=[wbb], w=[scr2_b[e][sg][j]], q="pool")

        for s_ in range(nseq):
            pcf = pC_t[:].rearrange("p a n -> p (a n)")
            for tt in range(S // 128):
                st, stb = wst.next()
                P.dma(st[:, 0:D], x_d[s_ * S + tt * 128:s_ * S + (tt + 1) * 128, :], w=[stb])

                def fn_tr(h, st=st):
                    ins = None
                    for k in range(KC):
                        ins = h.transpose(out=pcf[:, k * 128:(k + 1) * 128], in_=st[:, k * 128:(k + 1) * 128], identity=ident_f)
                    return ins
                P.op("pe", fn_tr, r=[stb, con_b], w=[pC_b])
                t = tt // 4
                P.op("act", lambda h, tt=tt: h.copy(out=xT[:, :, tt * 128:(tt + 1) * 128],
                                                    in_=pcf.rearrange("p (k n) -> p k n", n=128)),
                     r=[pC_b], w=[xb[k][t] for k in range(KC)])
            if dbg is not None and dbg[0] == "xT":
                dbg_dump_xT()
                break

            P.op("pool", lambda h: h.memset(small[:, 16:17], 0.0), r=[], w=[Gb, hfb] + pre.b)
            rmsnorm(s_, 0, range(NT))
            UW = S + CW - 1
            uT = G[:, 0:KC * UW].rearrange("p (k n) -> p k n", n=UW)
            P.op("pool", lambda h: h.memset(uT[:, :, 0:CW - 1], 0.0), w=[Gb])
            w1v = conv_w1_d.rearrange("(k p) n -> p k n", p=128)
            for m in range(KC):
                wb, wbb = load_w([(0, w1v[:, :, m * 128:(m + 1) * 128], KC, 128),
                                  (KC * 128, w1v[:, :, D + m * 128:D + (m + 1) * 128], KC, 128)], 2 * KC * 128)
                wa = wb[:, 0:KC * 128].rearrange("p (k n) -> p k n", n=128)
                wg = wb[:, KC * 128:2 * KC * 128].rearrange("p (k n) -> p k n", n=128)
                for t in range(NT):
                    tsl = slice(t * TT, (t + 1) * TT)
                    pa, pab = pA.next()
                    pg, pgb = pA.next()
                    mm_group(pa, [(wa[:, k, :], A[:, k, tsl]) for k in range(KC)], r=[wbb, Ab[t]], w=[pab])
                    mm_group(pg, [(wg[:, k, :], A[:, k, tsl]) for k in range(KC)], r=[wbb, Ab[t]], w=[pgb])
                    sg, sgb = tmp.next()
                    P.op("act", lambda h, sg=sg, pg=pg, m=m: h.activation(out=sg, in_=pg, func=AF.Sigmoid,
                                                                            bias=vcol("cb1", KC + m), scale=1.0),
                         r=[pgb, vecs_b], w=[sgb])
                    P.op("dve", lambda h, sg=sg, pa=pa, m=m, t=t: h.scalar_tensor_tensor(
                        out=uT[:, m, CW - 1 + t * TT:CW - 1 + (t + 1) * TT], in0=pa, scalar=vcol("cb1", m), in1=sg,
                        op0=ALU.add, op1=ALU.mult), r=[pab, sgb, vecs_b], w=[Gb])
            for m in range(KC):
                d1, d1b = wbf.next()
                d2, d2b = wbf.next()
                dv1 = d1[:, 0:16 * 128].rearrange("p (j n) -> p j n", n=128)
                dv2 = d2[:, 0:15 * 128].rearrange("p (j n) -> p j n", n=128)

                def dg(j, dv1=dv1, dv2=dv2):
                    return dv1[:, j, :] if j < 16 else dv2[:, j - 16, :]

                def fn_dg(h, m=m, lo=0, hi=16, dg=dg):
                    ins = None
                    for j in range(lo, hi):
                        ins = h.tensor_scalar(out=dg(j), in0=ident_bf, scalar1=vcol("dww", m * CW + j),
                                              scalar2=None, op0=ALU.mult)
                    return ins
                P.op("pool", fn_dg, r=[cbf_b, vecs_b], w=[d1b])
                P.op("pool", lambda h, f=fn_dg: f(h, lo=16, hi=CW), r=[cbf_b, vecs_b], w=[d2b])
                for t in range(NT):
                    tsl = slice(t * TT, (t + 1) * TT)
                    pz, pzb = pA.next()
                    mm_group(pz, [(dg(j), uT[:, m, t * TT + j:t * TT + j + TT]) for j in range(CW)],
                             r=[d1b, d2b, Gb], w=[pzb])
                    P.op("act", lambda h, pz=pz, m=m, tsl=tsl: h.activation(
                        out=A[:, m, tsl], in_=pz, func=AF.Identity, bias=vcol("dwb", m), scale=1.0),
                        r=[pzb, vecs_b], w=[Ab[t]])
            for t in range(NT):
                tsl = slice(t * TT, (t + 1) * TT)
                zs_t, zsb = wst.next()
                zsq = zs_t.bitcast(BF16)[:, 0:KC * TT].rearrange("p (k n) -> p k n", n=TT)
                P.op("pool", lambda h, zsq=zsq, tsl=tsl: h.tensor_tensor(out=zsq, in0=A[:, :, tsl], in1=A[:, :, tsl], op=ALU.mult),
                     r=[Ab[t]], w=[zsb])
                p1, p1b = pB.next()
                p2, p2b = pB.next()
                mm_group(p1, [(ones_bf, A[:, k, tsl]) for k in range(KC)], r=[Ab[t], cbf_b], w=[p1b])
                mm_group(p2, [(ones_bf, zsq[:, k, :]) for k in range(KC)], r=[zsb, cbf_b], w=[p2b])
                mean, meanb = p1, p1b
                rstd, rstdb = p2, p2b
                msq, msqb = tmp.next()
                P.op("act", lambda h, mean=mean, p1=p1: h.activation(out=mean, in_=p1, func=AF.Copy, scale=1.0 / D), r=[p1b], w=[meanb])
                P.op("act", lambda h, mean=mean, msq=msq: h.activation(out=msq, in_=mean, func=AF.Square), r=[meanb], w=[msqb])
                P.op("dve", lambda h, rstd=rstd, p2=p2, msq=msq: h.scalar_tensor_tensor(
                    out=rstd, in0=p2, scalar=1.0 / D, in1=msq, op0=ALU.mult, op1=ALU.subtract), r=[p2b, msqb], w=[rstdb])
                P.op("act", lambda h, rstd=rstd: h.activation(out=rstd, in_=rstd, func=AF.Sqrt, bias=EPS, scale=1.0), r=[rstdb], w=[rstdb])
                P.op("dve", lambda h, rstd=rstd: h.reciprocal(out=rstd, in_=rstd), r=[rstdb], w=[rstdb])
                for m in range(KC):
                    t1, t1b = tmp.next()
                    P.op("dve", lambda h, t1=t1, m=m, tsl=tsl, mean=mean: h.tensor_tensor(
                        out=t1, in0=A[:, m, tsl], in1=mean, op=ALU.subtract), r=[Ab[t], meanb], w=[t1b])
                    P.op("dve", lambda h, t1=t1, rstd=rstd: h.tensor_tensor(out=t1, in0=t1, in1=rstd, op=ALU.mult),
                         r=[rstdb], w=[t1b])
                    P.op("act", lambda h, t1=t1, m=m, t=t: h.activation(
                        out=uT[:, m, CW - 1 + t * TT:CW - 1 + (t + 1) * TT], in_=t1, func=AF.Silu,
                        bias=vcol("lnb", m), scale=vcol("lng", m)), r=[t1b, vecs_b], w=[Gb])
            w2cv = conv_w2_d.rearrange("(k p) n -> p k n", p=128)
            for j in range(KC):
                wb, wbb = load_w([(0, w2cv[:, :, j * 128:(j + 1) * 128], KC, 128)], KC * 128)
                w2b = wb[:, 0:KC * 128].rearrange("p (k n) -> p k n", n=128)
                for t in range(NT):
                    tsl = slice(t * TT, (t + 1) * TT)
                    py, pyb = pA.next()
                    mm_group(py, [(w2b[:, m, :], uT[:, m, CW - 1 + t * TT:CW - 1 + (t + 1) * TT]) for m in range(KC)],
                             r=[wbb, Gb], w=[pyb])
                    t1, t1b = tmp.next()
                    P.op("act", lambda h, t1=t1, py=py, j=j: h.activation(out=t1, in_=py, func=AF.Identity,
                                                                           bias=vcol("cb2", j), scale=1.0),
                         r=[pyb, vecs_b], w=[t1b])
                    P.op("dve", lambda h, t1=t1, j=j, tsl=tsl, s_=s_: h.scalar_tensor_tensor(
                        out=xT[:, j, tsl], in0=t1, scalar=modv[:, 16 + j, s_:s_ + 1], in1=xT[:, j, tsl],
                        op0=ALU.mult, op1=ALU.add), r=[t1b, modv_b, xb[j][t]], w=[xb[j][t]])
            if dbg is not None and dbg[0] == "x1":
                dbg_dump_xT()
                break
            rmsnorm(s_, 1, range(NT))
            l13, l2 = f32_loaders(ffn_w13_d, ffn_w2_d, FF0)
            ffn(l13, l2, FF0, 11, list(range(NT)), resid_epilogue(40, s_))
            if dbg is not None and dbg[0] == "x2":
                dbg_dump_xT()
                break

            rmsnorm(s_, 2, range(NT))
            rotary_tables(s_)
            if dbg is not None and dbg[0] == "kv0":
                dbg_dump_xT()
                break
            wkvv = w_kv_d.rearrange("(k p) n -> p k n", p=128)
            kbr = Ring(G[:, 0:4 * 1024].rearrange("p (s n) -> p s n", n=1024), 4, "kbr")
            P.op("pool", lambda h: h.memset(small[:, 16:17], 0.0), r=[Gb], w=[Gb] + kbr.b)
            wk_cur = [None]

            def k_stage_a(hd, t):
                if t == 0:
                    wb, wbb = load_w([(0, wkvv[:, :, hd * 128:(hd + 1) * 128], KC, 128)], KC * 128)
                    wk_cur[0] = (wb[:, 0:KC * 128].rearrange("p (k n) -> p k n", n=128), wbb)
                wk, wbb = wk_cur[0]
                tsl = slice(t * TT, (t + 1) * TT)
                pk, pkb = pA.next()
                mm_group(pk, [(wk[:, k, :], A[:, k, tsl]) for k in range(KC)], r=[wbb, Ab[t]], w=[pkb])
                kf, kfb = tmp.next()
                P.op("act", lambda h: h.copy(out=kf, in_=pk), r=[pkb], w=[kfb])
                return (hd, t, kf, kfb)

            def k_stage_b(item):
                hd, t, kf, kfb = item
                tsl = slice(t * TT, (t + 1) * TT)
                apply_rotary(kf, kfb, t)
                kb_t, kbb = kbr.next()
                kb16 = kb_t[:, 0:TT]
                ksq = kb_t[:, TT:2 * TT]
                sm, smb = rt.next()
                P.op("dve", lambda h: h.tensor_reduce(
                    out=sm[:, 0:2], in_=kf.rearrange("p (b n) -> p b n", n=256), axis=AX.X, op=ALU.add),
                    r=[kfb], w=[smb])
                P.op("dve", lambda h: h.tensor_scalar(
                    out=kmb[:, hd, 2 * t:2 * t + 2], in0=sm[:, 0:2], scalar1=1.0 / 256, scalar2=None, op0=ALU.mult),
                    r=[smb], w=[kmb_b])
                P.op("dve", lambda h: h.tensor_copy(out=kb16, in_=kf), r=[kfb], w=[kbb])
                P.op("pool", lambda h: h.tensor_tensor(out=ksq, in0=kf, in1=kf, op=ALU.mult), r=[kfb], w=[kbb])
                P.dma(k_scr[hd * 128:(hd + 1) * 128, tsl], kb16, r=[kbb], w=[kscr_b[hd][t]], q="pool")
                return (hd, t, ksq, kbb)

            def k_stage_c(item):
                hd, t, ksq, kbb = item
                pq, pqb = pA.next()
                P.op("pe", lambda h: h.matmul(pq, lhsT=ones_bf, rhs=ksq, start=True, stop=True),
                     r=[kbb, cbf_b], w=[pqb])
                if t == 0:
                    P.op("dve", lambda h: h.tensor_reduce(out=nhk[:, hd:hd + 1], in_=pq, axis=AX.X, op=ALU.max),
                         r=[pqb], w=[nhk_b])
                else:
                    sm2, sm2b = rt.next()
                    P.op("dve", lambda h: h.tensor_reduce(out=sm2[:, 0:1], in_=pq, axis=AX.X, op=ALU.max),
                         r=[pqb], w=[sm2b])
                    P.op("dve", lambda h: h.tensor_tensor(
                        out=nhk[:, hd:hd + 1], in0=nhk[:, hd:hd + 1], in1=sm2[:, 0:1], op=ALU.max), r=[sm2b], w=[nhk_b])
                if t == NT - 1:
                    P.op("dve", lambda h: h.tensor_scalar(
                        out=nhk[:, hd:hd + 1], in0=nhk[:, hd:hd + 1], scalar1=-0.5, scalar2=None, op0=ALU.mult), w=[nhk_b])

            a_q, b_q = [], []
            for hd in range(NH):
                for t in range(NT):
                    a_q.append(k_stage_a(hd, t))
                    if len(a_q) > 1:
                        b_q.append(k_stage_b(a_q.pop(0)))
                    if len(b_q) > 1:
                        k_stage_c(b_q.pop(0))
            while a_q:
                b_q.append(k_stage_b(a_q.pop(0)))
                if len(b_q) > 1:
                    k_stage_c(b_q.pop(0))
            while b_q:
                k_stage_c(b_q.pop(0))
            if dbg is not None and dbg[0] == "kvK":
                dbg_dump_xT()
                break
            wvb = G[:, 0:KC * D].rearrange("p (k n) -> p k n", n=D)
            P.op("pool", lambda h: h.memset(small[:, 16:17], 0.0), r=kbr.b, w=[Gb] + kbr.b)
            for k in range(KC):
                st, stb = wst.next()
                P.dma(st[:, 0:D], w_kv_d[k * 128:(k + 1) * 128, D:2 * D], w=[stb])
                P.op("pool", lambda h, st=st, k=k: h.tensor_copy(out=wvb[:, k, :], in_=st[:, 0:D]), r=[stb], w=[Gb])
            for tt in range(S // 128):
                t = tt // 4

                def fn_v(h, tt=tt):
                    ins = None
                    for half in range(2):
                        for k in range(KC):
                            ins = h.matmul(pC_t[:, half, :], lhsT=A[:, k, tt * 128:(tt + 1) * 128],
                                           rhs=wvb[:, k, half * TT:(half + 1) * TT], start=(k == 0), stop=(k == KC - 1))
                    return ins
                P.op("pe", fn_v, r=[Ab[t], Gb], w=[pC_b])
                vb_t, vbb = wbf.next()
                P.op("act", lambda h, vb_t=vb_t: h.copy(out=vb_t[:, 0:D], in_=pcf), r=[pC_b], w=[vbb])
                P.dma(v_scr[tt * 128:(tt + 1) * 128, :], vb_t[:, 0:D], r=[vbb], w=[vscr_b[tt]], q="pool")

            if dbg is not None and dbg[0] == "kvV":
                dbg_dump_xT()
                break
            rmsnorm(s_, 3, range(NT))
            OT = G[:, 0:NH * S].rearrange("p (k n) -> p k n", n=S)
            o0 = NH * S
            qT = G[:, o0:o0 + S]
            qsq = G[:, o0 + S:o0 + 2 * S]
            kTh = G[:, o0 + 2 * S:o0 + 3 * S]
            vH = G[:, o0 + 3 * S:o0 + 4 * S].rearrange("p (c n) -> p c n", n=128)
            OTb, qb_, kTb, vHb = Buf("OT"), Buf("q"), Buf("kT"), Buf("vH")
            P.op("pool", lambda h: h.memset(small[:, 16:17], 0.0), r=[Gb], w=[Gb, OTb, qb_, kTb, vHb, pC_b, pC0b, pC1b])
            wqv = w_q_d.rearrange("(k p) n -> p k n", p=128)
            for hd in range(NH):
                wb, wbb = load_w([(0, wqv[:, :, hd * 128:(hd + 1) * 128], KC, 128)], KC * 128)
                wq = wb[:, 0:KC * 128].rearrange("p (k n) -> p k n", n=128)
                P.dma(kTh, k_scr[hd * 128:(hd + 1) * 128, :], r=kscr_b[hd], w=[kTb])
                P.dma(vH, v_scr.rearrange("(c p) n -> p c n", p=128)[:, :, hd * 128:(hd + 1) * 128], r=vscr_b, w=[vHb])
                for t in range(NT):
                    tsl = slice(t * TT, (t + 1) * TT)
                    pk, pkb = pA.next()
                    mm_group(pk, [(wq[:, k, :], A[:, k, tsl]) for k in range(KC)], r=[wbb, Ab[t]], w=[pkb])
                    kf, kfb = tmp.next()
                    P.op("act", lambda h, kf=kf, pk=pk: h.copy(out=kf, in_=pk), r=[pkb], w=[kfb])
                    apply_rotary(kf, kfb, t)
                    P.op("dve", lambda h, kf=kf, tsl=tsl: h.tensor_copy(out=qT[:, tsl], in_=kf), r=[kfb], w=[qb_])
                    P.op("pool", lambda h, kf=kf, tsl=tsl: h.tensor_tensor(out=qsq[:, tsl], in0=kf, in1=kf, op=ALU.mult),
                         r=[kfb], w=[qb_])
                def g_stage1(qc, hd=hd):
                    b = qc // 2
                    qs = slice(qc * 128, (qc + 1) * 128)
                    pg, pgb = pA.next()

                    def fn_g(h):
                        h.matmul(pg[:, 0:8], lhsT=qT[:, qs], rhs=kmb[:, hd, :], start=True, stop=True)
                        return h.matmul(pg[:, 8:9], lhsT=qsq[:, qs], rhs=ones_bf[:, 0:1], start=True, stop=True)
                    P.op("pe", fn_g, r=[qb_, kmb_b, cbf_b], w=[pgb])
                    rtt, rtb = rt.next()
                    o_sm = CL["smask"][0] + b * 8
                    sm, smb = tmp.next()
                    gm = sm[:, 0:8]
                    t8 = sm[:, 8:16]
                    chain("dve", [
                        lambda h: h.tensor_tensor(out=gm, in0=pg[:, 0:8], in1=con[:, o_sm:o_sm + 8], op=ALU.add),
                        lambda h: h.max(out=t8, in_=gm),
                        lambda h: h.tensor_scalar(out=rtt[:, 0:8], in0=gm, scalar1=t8[:, 2:3], scalar2=BIG,
                                                  op0=ALU.is_ge, op1=ALU.mult),
                        lambda h: h.tensor_scalar(out=rtt[:, 8:9], in0=pg[:, 8:9], scalar1=-0.5,
                                                  scalar2=nhk[:, hd:hd + 1], op0=ALU.mult, op1=ALU.add),
                        lambda h: h.memset(rtt[:, 9:10], -BIG),
                    ], r=[pgb, con_b, nhk_b], w=[rtb, smb])
                    return (qs, rtt, rtb)

                def g_stage2(item):
                    qs, rtt, rtb = item
                    ptr, ptrb = pA.next()
                    P.op("pe", lambda h: h.transpose(out=ptr[0:10, 0:128], in_=rtt[:, 0:10], identity=ident_f),
                         r=[rtb, con_b], w=[ptrb])
                    P.op("act", lambda h: h.copy(out=rall[:, qs], in_=ptr[0:10, 0:128]), r=[ptrb], w=[rall_b])

                g_q = []
                for qc in range(S // 128):
                    g_q.append(g_stage1(qc))
                    if len(g_q) > 1:
                        g_stage2(g_q.pop(0))
                while g_q:
                    g_stage2(g_q.pop(0))
                if dbg is not None and dbg[0] == "h0g":
                    break
                acc_sets = [(pB_t[:, 0], pB.b[0], pB_t[:, 1], pB.b[1]), (pC_t[:, 0], pC0b, pC_t[:, 1], pC1b)]

                def s_stage(b, kc):
                    q0 = b * 256
                    n = kc // 2
                    qlo = 128 if kc == 2 * b + 1 else 0
                    var = n if n < b else 8
                    W_ = 256 - qlo
                    pss, pssb = pA.next()

                    def fn_s(h):
                        h.matmul(pss[:, 0:W_], lhsT=kTh[:, kc * 128:(kc + 1) * 128], rhs=qT[:, q0 + qlo:q0 + 256],
                                 start=True, stop=False)
                        return h.matmul(pss[:, 0:W_], lhsT=esel_bf[0:10, var * 128:(var + 1) * 128],
                                        rhs=rall[:, q0 + qlo:q0 + 256], start=False, stop=True)
                    P.op("pe", fn_s, r=[kTb, qb_, rall_b, cbf_b], w=[pssb])
                    ptile, ptb = pt.next()
                    P.op("act", lambda h: h.activation(out=ptile[:, 0:W_], in_=pss[:, 0:W_], func=AF.Exp, scale=ATT_SCALE),
                         r=[pssb], w=[ptb])
                    if kc >= 2 * b:
                        P.op("pool", lambda h: h.tensor_tensor(out=ptile[:, 0:128], in0=ptile[:, 0:128], in1=tri_bf, op=ALU.mult),
                             r=[cbf_b], w=[ptb])
                    return (b, kc, qlo, W_, ptile, ptb)

                def pv_stage(item, hd=hd):
                    b, kc, qlo, W_, ptile, ptb = item
                    nkc = 2 * b + 2
                    q0 = b * 256
                    po, pob, psm, psmb = acc_sets[b % 2]

                    def fn_pv(h):
                        h.matmul(po[:, qlo:256], lhsT=vH[:, kc, :], rhs=ptile[:, 0:W_], start=(kc == 0), stop=(kc == nkc - 1))
                        return h.matmul(psm[:, qlo:256], lhsT=ones_bf, rhs=ptile[:, 0:W_], start=(kc == 0), stop=(kc == nkc - 1))
                    P.op("pe", fn_pv, r=[ptb, vHb, cbf_b], w=[pob, psmb])
                    if kc == nkc - 1:
                        rs, rsb = tmp.next()
                        P.op("dve", lambda h: h.reciprocal(out=rs[:, 0:256], in_=psm[:, 0:256]), r=[psmb], w=[rsb])
                        P.op("dve", lambda h: h.tensor_tensor(
                            out=OT[:, hd, q0:q0 + 256], in0=po[:, 0:256], in1=rs[:, 0:256], op=ALU.mult),
                            r=[rsb, pob], w=[OTb])

                inflight = []
                for b in range(S // 256):
                    for kc in range(2 * b + 2):
                        inflight.append(s_stage(b, kc))
                        if len(inflight) > ATT_DEPTH:
                            pv_stage(inflight.pop(0))
                while inflight:
                    pv_stage(inflight.pop(0))
            if dbg is not None and dbg[0] == "h0g":
                dbg_dump_xT()
                break
            if dbg is not None and dbg[0] == "OT":
                for hd in range(NH):
                    st, stb = wst.next()
                    P.op("act", lambda h, st=st, hd=hd: h.copy(out=st[:, 0:S], in_=OT[:, hd, :]), r=[OTb], w=[stb])
                    b_ = Buf()
                    P.dma(out_d[:, hd * S:(hd + 1) * S], st[:, 0:S], r=[stb], w=[b_])
                    out_bufs.append(b_)
                break
            wov = w_o_d.rearrange("(k p) n -> p k n", p=128)
            for j in range(KC):
                wb, wbb = load_w([(0, wov[:, :, j * 128:(j + 1) * 128], KC, 128)], KC * 128)
                wo = wb[:, 0:KC * 128].rearrange("p (k n) -> p k n", n=128)
                for t in range(NT):
                    tsl = slice(t * TT, (t + 1) * TT)
                    py, pyb = pA.next()
                    mm_group(py, [(wo[:, hd, :], OT[:, hd, tsl]) for hd in range(NH)], r=[wbb, OTb], w=[pyb])
                    P.op("dve", lambda h, py=py, j=j, tsl=tsl, s_=s_: h.scalar_tensor_tensor(
                        out=xT[:, j, tsl], in0=py, scalar=modv[:, 48 + 16 + j, s_:s_ + 1], in1=xT[:, j, tsl],
                        op0=ALU.mult, op1=ALU.add), r=[pyb, modv_b, xb[j][t]], w=[xb[j][t]])
            if dbg is not None and dbg[0] == "x3":
                dbg_dump_xT()
                break

            P.op("pool", lambda h: h.memset(small[:, 16:17], 0.0), r=[], w=[Gb, hfb, OTb, qb_, kTb, vHb, pC_b, pC0b, pC1b])
            for t in range(NT):
                rmsnorm(s_, 4, [t], f32=True)
                for c4 in range(4):
                    qc = t * 4 + c4
                    pl, plb = pB.next()

                    def fn_r(h, pl=pl, c4=c4):
                        for k in range(KC):
                            h.matmul(pl[:, 0:NE], lhsT=hfv[:, k, c4 * 128:(c4 + 1) * 128], rhs=rwf[:, k, :], start=(k == 0), stop=False)
                        return h.matmul(pl[:, 0:NE], lhsT=ones_f[0:1, :], rhs=rbf[0:1, :], start=False, stop=True)
                    P.op("pe", fn_r, r=[hfb, rw_b, ones_fb], w=[plb])
                    sm, smb = tmp.next()
                    lg, t8, nt_, ee, sel, den = sm[:, 0:8], sm[:, 8:16], sm[:, 16:17], sm[:, 24:32], sm[:, 32:40], sm[:, 40:41]
                    chain("dve", [
                        lambda h, lg=lg, pl=pl: h.tensor_copy(out=lg, in_=pl[:, 0:NE]),
                        lambda h, lg=lg, t8=t8: h.max(out=t8, in_=lg),
                        lambda h, t8=t8, nt_=nt_: h.tensor_scalar(out=nt_, in0=t8[:, 0:1], scalar1=-1.0, scalar2=None, op0=ALU.mult),
                        lambda h, lg=lg, t8=t8, sel=sel: h.tensor_scalar(out=sel, in0=lg, scalar1=t8[:, 1:2], scalar2=None, op0=ALU.is_ge),
                    ], r=[plb], w=[smb])
                    P.op("act", lambda h, lg=lg, ee=ee, nt_=nt_: h.activation(out=ee, in_=lg, func=AF.Exp, bias=nt_, scale=1.0),
                         r=[smb], w=[smb])
                    chain("dve", [
                        lambda h, ee=ee, sel=sel: h.tensor_tensor(out=ee, in0=ee, in1=sel, op=ALU.mult),
                        lambda h, ee=ee, den=den: h.tensor_reduce(out=den, in_=ee, axis=AX.X, op=ALU.add),
                        lambda h, den=den: h.reciprocal(out=den, in_=den),
                    ], r=[], w=[smb])
                    P.op("dve", lambda h, ee=ee, den=den, qc=qc: h.tensor_scalar(out=gates[:, qc, :], in0=ee, scalar1=den, scalar2=None,
                                                                               op0=ALU.mult), r=[smb], w=[gates_b])
            gbufs = [G32[:, 8192:8192 + S], G32[:, 8192 + S:8192 + 2 * S]]
            gbbs = [Buf("gb0"), Buf("gb1")]
            P.op("pool", lambda h: h.memset(small[:, 16:17], 0.0), r=[Gb, hfb], w=[Gb, hfb] + gbbs)

            def build_gb(e):
                gb, gbb = gbufs[e % 2], gbbs[e % 2]
                for qc in range(S // 128):
                    g1, g1b = tmp.next()
                    P.op("dve", lambda h, g1=g1, qc=qc: h.tensor_scalar(
                        out=g1[:, 0:128], in0=ones_f[:], scalar1=gates[:, qc, e:e + 1], scalar2=None, op0=ALU.mult),
                        r=[gates_b, ones_fb], w=[g1b])
                    pgt, pgtb = pB.next()
                    P.op("pe", lambda h, pgt=pgt, g1=g1: h.transpose(out=pgt[:, 0:128], in_=g1[:, 0:128], identity=ident_f),
                         r=[g1b, con_b], w=[pgtb])
                    P.op("act", lambda h, pgt=pgt, qc=qc: h.copy(out=gb[:, qc * 128:(qc + 1) * 128], in_=pgt[:, 0:128]),
                         r=[pgtb], w=[gbb])

            build_gb(0)
            for e in range(NE):
                gb, gbb = gbufs[e % 2], gbbs[e % 2]

                def moe_ep(j, t, ti, py, pyb, gb=gb, gbb=gbb, s_=s_):
                    tsl = slice(t * TT, (t + 1) * TT)
                    t1, t1b = tmp.next()
                    P.op("dve", lambda h: h.scalar_tensor_tensor(
                        out=t1, in0=py, scalar=modv[:, 48 + 40 + j, s_:s_ + 1], in1=gb[:, t * TT:(t + 1) * TT],
                        op0=ALU.mult, op1=ALU.mult), r=[pyb, modv_b, gbb], w=[t1b])
                    P.op("pool", lambda h: h.tensor_tensor(out=xT[:, j, tsl], in0=xT[:, j, tsl], in1=t1, op=ALU.add),
                         r=[t1b, xb[j][t]], w=[xb[j][t]])

                def l13(c, e=e):
                    wb, wbb = wbf.next()
                    r0 = (e * MOE_NCH + c) * 128
                    P.dma(wb[:, 0:2 * KC * 128], moe_scr13[r0:r0 + 128, :], r=[scr13_b[e][c]], w=[wbb])
                    return wb, wbb

                def l2(c0, seg, j, e=e):
                    wb, wbb = wbf.next()
                    sg = c0 // MOE_SEG
                    r0 = ((e * MOE_NSEG + sg) * KC + j) * 128
                    P.dma(wb[:, 0:MOE_SEG * 128], moe_scr2[r0:r0 + 128, :], r=[scr2_b[e][sg][j]], w=[wbb])
                    return wb, wbb
                hook = (lambda e=e: build_gb(e + 1)) if e + 1 < NE else None
                ffn(l13, l2, FFE, MOE_SEG, list(range(NT)), moe_ep, mid_hook=hook)
            if dbg is not None and dbg[0] == "x4":
                dbg_dump_xT()
                break

            P.op("pool", lambda h: h.memset(small[:, 16:17], 0.0), r=[], w=[Gb, hfb] + gbbs)
            for t in range(NT):
                rmsnorm(s_, None, [t])
                for c4 in range(4):
                    def fn_to(h, c4=c4):
                        ins = None
                        for k in range(KC):
                            ins = h.transpose(out=pcf[:, k * 128:(k + 1) * 128], in_=hfv[:, k, c4 * 128:(c4 + 1) * 128], identity=ident_f)
                        return ins
                    P.op("pe", fn_to, r=[hfb, con_b], w=[pC_b])
                    st, stb = wst.next()
                    P.op("act", lambda h, st=st: h.copy(out=st[:, 0:D], in_=pcf), r=[pC_b], w=[stb])
                    r0 = s_ * S + t * TT + c4 * 128
                    b_ = Buf()
                    P.dma(out_d[r0:r0 + 128, :], st[:, 0:D], r=[stb], w=[b_], q="pool")
                    out_bufs.append(b_)

        P.op("sp", lambda h: h.nop(), r=out_bufs)
        P.emit()
    return nc


def _prep_shared(inp):
    VL = _vec_layout()
    vecs = np.zeros((128, VL["_n"][0]), np.float32)

    def put(name, v):
        o, n = VL[name]
        vecs[:, o:o + n] = _cols(v)
    put("ada_b0", inp["ada_b"][0]); put("ada_b1", inp["ada_b"][1]); put("kv_ada_b", inp["kv_ada_b"])
    put("n1g0", inp["norm1_g"][0]); put("n1g1", inp["norm1_g"][1])
    put("n2g0", inp["norm2_g"][0]); put("n2g1", inp["norm2_g"][1])
    put("kvg", inp["kv_norm_g"]); put("fing", inp["final_g"])
    put("cb1", inp["conv_b1"][0]); put("dwb", inp["conv_dw_b"][0])
    put("lng", inp["conv_ln_g"][0]); put("lnb", inp["conv_ln_b"][0]); put("cb2", inp["conv_b2"][0])
    o, n = VL["dww"]
    dw = np.asarray(inp["conv_dw_w"][0], np.float32)
    for m in range(KC):
        vecs[:, o + m * CW:o + (m + 1) * CW] = dw[:, m * 128:(m + 1) * 128].T
    c1, c2 = _make_consts()
    f = lambda a, shp: np.ascontiguousarray(np.asarray(a, np.float32).reshape(shp))
    return {
        "vecs": vecs, "consts": c1, "consts2": c2,
        "ada_w": f(inp["ada_w"], (2 * D, 6 * D)), "kv_ada_w": f(inp["kv_ada_w"], (D, 2 * D)),
        "conv_w1": f(inp["conv_w1"], (D, 2 * D)), "conv_w2": f(inp["conv_w2"], (D, D)),
        "w_kv": f(inp["w_kv"], (D, 2 * D)), "w_q": f(inp["w_q"], (D, D)), "w_o": f(inp["w_o"], (D, D)),
        "ffn_w13": f(inp["ffn_w13"], (D, 2 * FF0)), "ffn_w2": f(inp["ffn_w2"], (FF0, D)),
        "router_w": f(inp["router_w"], (D, NE)), "router_b": f(inp["router_b"], (1, NE)),
        "moe_w13": f(inp["moe_w13"], (NE * D, 2 * FFE)), "moe_w2": f(inp["moe_w2"], (NE * FFE, D)),
    }


def _core_inputs(inp, shared, seqs):
    nseq = len(seqs)
    x = np.ascontiguousarray(np.asarray(inp["x"], np.float32)[seqs].reshape(nseq * S, D))
    c = np.asarray(inp["c"], np.float32)[seqs]
    cT = np.ascontiguousarray(c.reshape(nseq, KC, 128).transpose(2, 1, 0).reshape(128, KC * nseq))
    pos = np.ascontiguousarray(np.asarray(inp["positions"], np.int32)[seqs])
    m = dict(shared)
    m.update({"x": x, "cT": cT, "pos": pos})
    return m


def run(inp, n_cores, nseq, dbg=None, seq0=0):
    nc = build(nseq, dbg)
    shared = _prep_shared(inp)
    in_maps = [_core_inputs(inp, shared, list(range(seq0 + c * nseq, seq0 + (c + 1) * nseq))) for c in range(n_cores)]
    res = run_bass_kernel_spmd(nc, in_maps, core_ids=list(range(n_cores)))
    return [r["out"] for r in res.results]


def kernel(**inputs):
    nseq = 32 // N_CORES
    outs = run(inputs, N_CORES, nseq)
    return np.concatenate([o.reshape(nseq, S, D) for o in outs], axis=0).astype(np.float32)
```

```python
import math
from contextlib import ExitStack

import numpy as np
import concourse.bass as bass
import concourse.mybir as mybir
from concourse.bass_utils import run_bass_kernel_spmd

F32 = mybir.dt.float32
BF16 = mybir.dt.bfloat16
I32 = mybir.dt.int32
AF = mybir.ActivationFunctionType
ALU = mybir.AluOpType
AX = mybir.AxisListType

D = 1024
S = 2048
KC = 8
NH = 8
TT = 512
NT = S // TT
FF0 = 2816
FFE = 3584
NE = 8
CW = 31
EPS = 1e-6
BIG = 30000.0
ATT_SCALE = 128 ** -0.5
N_CORES = 8
ATT_DEPTH = 2
MOE_NCH = FFE // 128
MOE_SEG = 7
MOE_NSEG = MOE_NCH // MOE_SEG

ENGS = ("pe", "act", "dve", "pool", "sp")


class Buf:
    __slots__ = ("name", "w", "r")

    def __init__(self, name=""):
        self.name = name
        self.w = None
        self.r = []


class Op:
    __slots__ = ("eng", "fn", "deps", "signal", "count", "dma", "slot", "val")

    def __init__(self, eng, fn, deps, dma=False):
        self.eng = eng
        self.fn = fn
        self.deps = deps
        self.signal = False
        self.count = 0
        self.dma = dma
        self.slot = None
        self.val = 0


class Prog:
    NSLOT = 8

    def __init__(self, nc, es):
        self.nc = nc
        self.ops = {e: [] for e in ENGS}
        self.sem = {e: es.enter_context(nc.semaphore("sem_" + e)) for e in ENGS}
        self.dq = {q: [es.enter_context(nc.semaphore("dq_%s%d" % (q, i))) for i in range(self.NSLOT)] for q in ("sp", "pool")}
        self.ndma = {"sp": 0, "pool": 0}

    @staticmethod
    def _deps(r, w):
        deps = []
        for b in r:
            if b.w is not None:
                deps.append(b.w)
        for b in w:
            if b.w is not None:
                deps.append(b.w)
            deps.extend(b.r)
        return deps

    @staticmethod
    def _commit(op, r, w):
        for b in r:
            b.r.append(op)
        for b in w:
            b.w = op
            b.r = []

    def op(self, eng, fn, r=(), w=()):
        o = Op(eng, fn, self._deps(r, w))
        self.ops[eng].append(o)
        self._commit(o, r, w)
        return o

    def dma(self, out, in_, r=(), w=(), q="sp"):
        o = Op(q, (out, in_), self._deps(r, w), dma=True)
        i = self.ndma[q]
        self.ndma[q] += 1
        o.slot = i % self.NSLOT
        o.val = 16 * (i // self.NSLOT + 1)
        self.ops[q].append(o)
        self._commit(o, r, w)
        return o

    def emit(self):
        nc = self.nc
        for e in ENGS:
            for o in self.ops[e]:
                for d in o.deps:
                    if not d.dma:
                        d.signal = True
        for e in ENGS:
            c = 0
            for o in self.ops[e]:
                if not o.dma and o.signal:
                    c += 1
                    o.count = c

        def run(e, h):
            seen = {}
            for o in self.ops[e]:
                waits = {}
                for d in o.deps:
                    if d.dma:
                        key = ("d", d.eng, d.slot)
                        v = d.val
                    else:
                        key = ("e", d.eng)
                        v = d.count
                    if v > waits.get(key, 0):
                        waits[key] = v
                if o.dma and o.val > 16:
                    key = ("d", o.eng, o.slot)
                    waits[key] = max(waits.get(key, 0), o.val - 16)
                for key, v in waits.items():
                    if seen.get(key, 0) >= v:
                        continue
                    seen[key] = v
                    s = self.dq[key[1]][key[2]] if key[0] == "d" else self.sem[key[1]]
                    h.wait_ge(s, v)
                if o.dma:
                    out, in_ = o.fn
                    h.dma_start(out=out, in_=in_).then_inc(self.dq[o.eng][o.slot], 16)
                else:
                    ins = o.fn(h)
                    if o.signal:
                        ins.then_inc(self.sem[e], 1)

        with nc.Block() as block:
            @block.tensor
            def _(h):
                run("pe", h)

            @block.scalar
            def _(h):
                run("act", h)

            @block.vector
            def _(h):
                run("dve", h)

            @block.gpsimd
            def _(h):
                run("pool", h)

            @block.sync
            def _(h):
                run("sp", h)


class Ring:
    def __init__(self, t, n, name):
        self.t = t
        self.n = n
        self.b = [Buf("%s%d" % (name, i)) for i in range(n)]
        self.i = 0

    def next(self):
        i = self.i % self.n
        self.i += 1
        return self.t[:, i], self.b[i]


def _cols(v):
    v = np.asarray(v, np.float32).reshape(-1, 128)
    return np.ascontiguousarray(v.T)


VEC_LAYOUT = {}


def _vec_layout():
    if VEC_LAYOUT:
        return VEC_LAYOUT
    off = 0
    for name, n in (("ada_b0", 48), ("ada_b1", 48), ("kv_ada_b", 16), ("n1g0", 8), ("n1g1", 8),
                    ("n2g0", 8), ("n2g1", 8), ("kvg", 8), ("fing", 8), ("cb1", 16), ("dwb", 8),
                    ("lng", 8), ("lnb", 8), ("cb2", 8), ("dww", 8 * CW)):
        VEC_LAYOUT[name] = (off, n)
        off += n
    VEC_LAYOUT["_n"] = (off, 0)
    return VEC_LAYOUT


CONST_LAYOUT = {}


def _const_layout():
    if CONST_LAYOUT:
        return CONST_LAYOUT
    off = 0
    for name, n in (("ident", 128), ("smask", 64), ("invf", 1), ("sgn", 1), ("pm", 32)):
        CONST_LAYOUT[name] = (off, n)
        off += n
    CONST_LAYOUT["_n"] = (off, 0)
    return CONST_LAYOUT


NC2 = 12 * 128


def _make_consts():
    L = _const_layout()
    c = np.zeros((128, L["_n"][0]), np.float32)
    o = L["ident"][0]
    c[:, o:o + 128] = np.eye(128, dtype=np.float32)
    o = L["smask"][0]
    for b in range(8):
        for n in range(8):
            c[:, o + b * 8 + n] = -BIG if n >= b else 0.0
    o = L["invf"][0]
    i = np.arange(32)
    invf = 500000.0 ** (-(2.0 * (i % 16)) / 32.0)
    c[:32, o] = (invf / (2.0 * math.pi)).astype(np.float32)
    o = L["sgn"][0]
    c[:16, o] = -1.0
    c[16:32, o] = 1.0
    o = L["pm"][0]
    for col in range(32):
        partner = col + 16 if col < 16 else col - 16
        c[partner, o + col] = 1.0
    c2 = np.zeros((128, NC2), np.float32)
    c2[:, 0:128] = np.eye(128, dtype=np.float32)
    c2[:, 128:256] = 1.0
    kk = np.arange(128)[:, None]
    qq = np.arange(128)[None, :]
    c2[:, 256:384] = (kk <= qq).astype(np.float32)
    o = 384
    for n in range(8):
        for r in (n, 8, 9):
            c2[r, o + n * 128:o + (n + 1) * 128] = 1.0
    c2[8, o + 8 * 128:o + 9 * 128] = 1.0
    return c, c2


def build(nseq, dbg=None):
    nc = bass.Bass("TRN2", target_bir_lowering=False)
    VL = _vec_layout()
    CL = _const_layout()
    NTOK = nseq * S
    NVEC = VL["_n"][0]
    NCON = CL["_n"][0]

    def din(name, shape, dt=F32):
        return nc.dram_tensor(name, shape, dt, kind="ExternalInput").ap()

    x_d = din("x", [NTOK, D])
    cT_d = din("cT", [128, KC * nseq])
    pos_d = din("pos", [nseq, S], I32)
    vec_d = din("vecs", [128, NVEC])
    con_d = din("consts", [128, NCON])
    con2_d = din("consts2", [128, NC2])
    ada_w_d = din("ada_w", [2 * D, 6 * D])
    kv_ada_w_d = din("kv_ada_w", [D, 2 * D])
    conv_w1_d = din("conv_w1", [D, 2 * D])
    conv_w2_d = din("conv_w2", [D, D])
    w_kv_d = din("w_kv", [D, 2 * D])
    w_q_d = din("w_q", [D, D])
    w_o_d = din("w_o", [D, D])
    ffn_w13_d = din("ffn_w13", [D, 2 * FF0])
    ffn_w2_d = din("ffn_w2", [FF0, D])
    router_w_d = din("router_w", [D, NE])
    router_b_d = din("router_b", [1, NE])
    moe_w13_d = din("moe_w13", [NE * D, 2 * FFE])
    moe_w2_d = din("moe_w2", [NE * FFE, D])
    if dbg is None:
        out_d = nc.dram_tensor("out", [NTOK, D], F32, kind="ExternalOutput").ap()
    else:
        out_d = nc.dram_tensor("out", list(dbg[1]), F32, kind="ExternalOutput").ap()
    k_scr = nc.dram_tensor("k_scr", [NH * 128, S], BF16, kind="Internal").ap()
    v_scr = nc.dram_tensor("v_scr", [S, D], BF16, kind="Internal").ap()
    kscr_b = [[Buf("kscr%d_%d" % (h_, t_)) for t_ in range(NT)] for h_ in range(NH)]
    vscr_b = [Buf("vscr%d" % t_) for t_ in range(S // 128)]
    moe_scr13 = nc.dram_tensor("moe_scr13", [NE * MOE_NCH * 128, 2 * KC * 128], BF16, kind="Internal").ap()
    moe_scr2 = nc.dram_tensor("moe_scr2", [NE * MOE_NSEG * KC * 128, MOE_SEG * 128], BF16, kind="Internal").ap()
    scr13_b = [[Buf("s13_%d_%d" % (e, c)) for c in range(MOE_NCH)] for e in range(NE)]
    scr2_b = [[[Buf("s2_%d_%d_%d" % (e, sg, j)) for j in range(KC)] for sg in range(MOE_NSEG)] for e in range(NE)]
    out_bufs = []

    es = ExitStack()
    with es:
        P = Prog(nc, es)

        def sb(name, shape, dt=F32):
            return es.enter_context(nc.sbuf_tensor(name, shape, dt))

        def ps(name, shape, dt=F32):
            return es.enter_context(nc.psum_tensor(name, shape, dt))

        xT = sb("xT", [128, KC, S])
        xb = [[Buf("x%d_%d" % (k, t)) for t in range(NT)] for k in range(KC)]
        A = sb("A", [128, KC, S], BF16)
        Ab = [Buf("A%d" % t) for t in range(NT)]
        G32 = sb("G", [128, 12288])
        G = G32[:].bitcast(BF16)
        Gb = Buf("G")
        WS = 2048
        wst_t = sb("wst", [128, 2, WS])
        wst = Ring(wst_t, 2, "wst")
        wbf_t = sb("wbf", [128, 3, WS], BF16)
        wbf = Ring(wbf_t, 3, "wbf")
        tmp_t = sb("tmpf", [128, 4, TT])
        tmp = Ring(tmp_t, 4, "tmp")
        vecs = sb("vecs_sb", [128, NVEC])
        vecs_b = Buf("vecs")
        con = sb("con_sb", [128, NCON])
        con_b = Buf("con")
        cbf = sb("cbf", [128, NC2], BF16)
        cbf_b = Buf("cbf")
        modv = sb("modv", [128, 112, nseq])
        modv_b = Buf("modv")
        gsv = sb("gsv", [128, 5, KC, nseq])
        gsv_b = Buf("gsv")
        sc = sb("sc", [128, KC * nseq])
        sc_b = Buf("sc")
        small = sb("small", [128, 64])
        ones_f = sb("ones_f", [128, 128])
        ones_fb = Buf("ones_f")
        rot_t = sb("rot", [32, 2, S], BF16)
        rot_b = Buf("rot")
        kmb = sb("kmb", [128, NH, 8], BF16)
        kmb_b = Buf("kmb")
        nhk = sb("nhk", [128, NH])
        nhk_b = Buf("nhk")
        gates = sb("gates", [128, S // 128, NE])
        gates_b = Buf("gates")
        rall = sb("rall", [10, S], BF16)
        rall_b = Buf("rall")
        rt_t = sb("rt", [128, 4, 16])
        rt = Ring(rt_t, 4, "rt")
        pt_t = sb("pt", [128, 4, 256], BF16)
        pt = Ring(pt_t, 4, "pt")
        rwf = sb("rwf", [128, KC, NE])
        rbf = sb("rbf", [1, NE])
        rw_b = Buf("rw")

        pA_t = ps("pA", [128, 4, TT])
        pA = Ring(pA_t, 4, "pA")
        pB_t = ps("pB", [128, 2, TT])
        pB = Ring(pB_t, 2, "pB")
        pC_t = ps("pC", [128, 2, TT])
        pC_b = Buf("pC")
        pC0b, pC1b = Buf("pC0"), Buf("pC1")

        ident_f = con[:, CL["ident"][0]:CL["ident"][0] + 128]
        ident_bf = cbf[:, 0:128]
        ones_bf = cbf[:, 128:256]
        tri_bf = cbf[:, 256:384]
        esel_bf = cbf[:, 384:384 + 9 * 128]

        def vcol(name, i):
            o = VL[name][0] + i
            return vecs[:, o:o + 1]

        def ccol(name, i=0, n=1, rows=128):
            o = CL[name][0] + i
            return con[0:rows, o:o + n]

        def chain(eng, fns, r=(), w=()):
            for f in fns:
                P.op(eng, f, r=r, w=w)

        P.dma(vecs[:], vec_d[:, :], w=[vecs_b])
        P.dma(con[:], con_d[:, :], w=[con_b])
        P.dma(sc[:], cT_d[:, :], w=[sc_b])
        P.dma(rwf[:], router_w_d.rearrange("(k p) n -> p k n", p=128), w=[rw_b])
        P.dma(rbf[:], router_b_d[:, :], w=[rw_b])
        P.op("pool", lambda h: h.memset(ones_f[:], 1.0), w=[ones_fb])
        st0, st0b = wst.next()
        P.dma(st0[:, 0:NC2], con2_d[:, :], w=[st0b])
        P.op("dve", lambda h: h.tensor_copy(out=cbf[:], in_=st0[:, 0:NC2]), r=[st0b], w=[cbf_b])
        P.op("act", lambda h: h.activation(out=sc[:], in_=sc[:], func=AF.Silu), r=[sc_b], w=[sc_b])

        cast_rr = [0]

        def load_w(pieces, ncols):
            wb, wbb = wbf.next()
            for (co, ap, kc, n) in pieces:
                st, stb = wst.next()
                P.dma(st[:, 0:kc * n].rearrange("p (k n) -> p k n", n=n), ap, w=[stb])
                P.op("pool", lambda h, st=st, co=co, kc=kc, n=n: h.tensor_copy(out=wb[:, co:co + kc * n], in_=st[:, 0:kc * n]),
                     r=[stb], w=[wbb])
            return wb, wbb

        def mm_group(out_ap, pairs, r, w):
            def fn(h):
                n = len(pairs)
                ins = None
                for i, (l, rr) in enumerate(pairs):
                    ins = h.matmul(out_ap, lhsT=l, rhs=rr, start=(i == 0), stop=(i == n - 1))
                return ins
            return P.op("pe", fn, r=r, w=w)

        def ada(w_ap, nchunks, bias_name, dst_off):
            wv = w_ap.rearrange("(k p) n -> p k n", p=128)
            for blk in range(nchunks // 2):
                st, stb = wst.next()
                stv = st[:, 0:KC * 256].rearrange("p (k n) -> p k n", n=256)
                P.dma(stv, wv[:, :, blk * 256:(blk + 1) * 256], w=[stb])
                for m in range(2):
                    idx = blk * 2 + m
                    pt_, pb_ = pA.next()
                    mm_group(pt_[:, 0:nseq],
                             [(stv[:, k, m * 128:(m + 1) * 128], sc[:, k * nseq:(k + 1) * nseq]) for k in range(KC)],
                             r=[stb, sc_b], w=[pb_])
                    P.op("act", lambda h, pt_=pt_, idx=idx: h.activation(
                        out=modv[:, dst_off + idx, :], in_=pt_[:, 0:nseq], func=AF.Identity,
                        bias=vcol(bias_name, idx), scale=1.0), r=[pb_, vecs_b], w=[modv_b])

        ada(ada_w_d[0:D, :], 48, "ada_b0", 0)
        ada(ada_w_d[D:2 * D, :], 48, "ada_b1", 48)
        ada(kv_ada_w_d, 16, "kv_ada_b", 96)
        GS_SRC = [(8, "n1g0"), (32, "n2g0"), (96 + 8, "kvg"), (48 + 8, "n1g1"), (48 + 32, "n2g1")]
        SHIFT_SRC = [0, 24, 96, 48, 48 + 24]
        for wi, (so, gname) in enumerate(GS_SRC):
            go = VL[gname][0]
            for s_ in range(nseq):
                P.op("dve", lambda h, wi=wi, so=so, go=go, s_=s_: h.scalar_tensor_tensor(
                    out=gsv[:, wi, :, s_], in0=modv[:, so:so + 8, s_], scalar=1.0,
                    in1=vecs[:, go:go + 8], op0=ALU.add, op1=ALU.mult), r=[modv_b, vecs_b], w=[gsv_b])

        sqv = G[:, 0:KC * TT].rearrange("p (k n) -> p k n", n=TT)
        hfv = G32[:, KC * TT // 2:KC * TT // 2 + KC * TT].rearrange("p (k n) -> p k n", n=TT)
        hfb = Buf("hf")

        def rmsnorm(s_, which, tiles, f32=False):
            for t in tiles:
                tsl = slice(t * TT, (t + 1) * TT)
                P.op("pool", lambda h, tsl=tsl: h.tensor_tensor(out=sqv, in0=xT[:, :, tsl], in1=xT[:, :, tsl], op=ALU.mult),
                     r=[xb[k][t] for k in range(KC)], w=[Gb])
                pss, pssb = pB.next()
                mm_group(pss, [(ones_bf, sqv[:, k, :]) for k in range(KC)], r=[Gb, cbf_b], w=[pssb])
                sd, sdb = pss, pssb
                P.op("act", lambda h, sd=sd, pss=pss: h.activation(out=sd, in_=pss, func=AF.Sqrt, bias=EPS, scale=1.0 / D),
                     r=[pssb], w=[sdb])
                P.op("dve", lambda h, sd=sd: h.reciprocal(out=sd, in_=sd), r=[sdb], w=[sdb])
                for k in range(KC):
                    if which is None:
                        P.op("dve", lambda h, k=k, sd=sd, tsl=tsl: h.scalar_tensor_tensor(
                            out=hfv[:, k, :], in0=xT[:, k, tsl], scalar=vcol("fing", k), in1=sd,
                            op0=ALU.mult, op1=ALU.mult), r=[xb[k][t], sdb, vecs_b], w=[hfb])
                        continue
                    t1, t1b = tmp.next()
                    P.op("dve", lambda h, k=k, sd=sd, t1=t1, tsl=tsl: h.scalar_tensor_tensor(
                        out=t1, in0=xT[:, k, tsl], scalar=gsv[:, which, k, s_:s_ + 1], in1=sd,
                        op0=ALU.mult, op1=ALU.mult), r=[xb[k][t], sdb, gsv_b], w=[t1b])
                    sh = modv[:, SHIFT_SRC[which] + k, s_:s_ + 1]
                    if f32:
                        P.op("act", lambda h, k=k, t1=t1, sh=sh: h.activation(
                            out=hfv[:, k, :], in_=t1, func=AF.Identity, bias=sh, scale=1.0),
                            r=[t1b, modv_b], w=[hfb])
                        P.op("pool", lambda h, k=k, tsl=tsl: h.tensor_copy(out=A[:, k, tsl], in_=hfv[:, k, :]),
                             r=[hfb], w=[Ab[t]])
                    else:
                        P.op("act", lambda h, k=k, t1=t1, sh=sh, tsl=tsl: h.activation(
                            out=A[:, k, tsl], in_=t1, func=AF.Identity, bias=sh, scale=1.0),
                            r=[t1b, modv_b], w=[Ab[t]])

        def ffn(load13, load2, ff, seg, tiles, epilogue):
            nch = ff // 128
            ntl = len(tiles)
            Gv = G[:, 0:seg * ntl * TT].rearrange("p (c n) -> p c n", n=ntl * TT)
            for c0 in range(0, nch, seg):
                for cc in range(seg):
                    c = c0 + cc
                    wb, wbb = load13(c)
                    wa = wb[:, 0:KC * 128].rearrange("p (k n) -> p k n", n=128)
                    wg = wb[:, KC * 128:2 * KC * 128].rearrange("p (k n) -> p k n", n=128)
                    for ti, t in enumerate(tiles):
                        tsl = slice(t * TT, (t + 1) * TT)
                        pa, pab = pA.next()
                        pg, pgb = pA.next()
                        mm_group(pa, [(wa[:, k, :], A[:, k, tsl]) for k in range(KC)], r=[wbb, Ab[t]], w=[pab])
                        mm_group(pg, [(wg[:, k, :], A[:, k, tsl]) for k in range(KC)], r=[wbb, Ab[t]], w=[pgb])
                        sa, sab = tmp.next()
                        P.op("act", lambda h, sa=sa, pa=pa: h.activation(out=sa, in_=pa, func=AF.Silu), r=[pab], w=[sab])
                        P.op("dve", lambda h, sa=sa, pg=pg, cc=cc, ti=ti: h.tensor_tensor(
                            out=Gv[:, cc, ti * TT:(ti + 1) * TT], in0=pg, in1=sa, op=ALU.mult), r=[sab, pgb], w=[Gb])
                for j in range(KC):
                    wb, wbb = load2(c0, seg, j)
                    w2b = wb[:, 0:seg * 128].rearrange("p (c n) -> p c n", n=128)
                    for ti, t in enumerate(tiles):
                        py, pyb = pA.next()
                        mm_group(py, [(w2b[:, cc, :], Gv[:, cc, ti * TT:(ti + 1) * TT]) for cc in range(seg)],
                                 r=[wbb, Gb], w=[pyb])
                        epilogue(j, t, ti, py, pyb)

        def f32_loaders(w13_ap, w2_ap, ff):
            w13v = w13_ap.rearrange("(k p) n -> p k n", p=128)
            w2v = w2_ap.rearrange("(c p) n -> p c n", p=128)

            def l13(c):
                return load_w([(0, w13v[:, :, c * 128:(c + 1) * 128], KC, 128),
                               (KC * 128, w13v[:, :, ff + c * 128:ff + (c + 1) * 128], KC, 128)], 2 * KC * 128)

            def l2(c0, seg, j):
                return load_w([(0, w2v[:, c0:c0 + seg, j * 128:(j + 1) * 128], seg, 128)], seg * 128)
            return l13, l2

        def resid_epilogue(gate_off, s_):
            def ep(j, t, ti, py, pyb):
                tsl = slice(t * TT, (t + 1) * TT)
                P.op("dve", lambda h: h.scalar_tensor_tensor(
                    out=xT[:, j, tsl], in0=py, scalar=modv[:, gate_off + j, s_:s_ + 1], in1=xT[:, j, tsl],
                    op0=ALU.mult, op1=ALU.add), r=[pyb, modv_b, xb[j][t]], w=[xb[j][t]])
            return ep

        def rotary_tables(s_):
            for t in range(NT):
                tsl = slice(t * TT, (t + 1) * TT)
                ya, yab = tmp.next()
                yb, ybb = tmp.next()
                yc, ycb = tmp.next()
                ya, yb, yc = ya[0:32, :], yb[0:32, :], yc[0:32, :]
                yai = ya.bitcast(I32)
                P.dma(yai, pos_d[s_:s_ + 1, tsl].to_broadcast([32, TT]), w=[yab])
                fns = [lambda h, yb=yb, yai=yai: h.tensor_copy(out=yb, in_=yai),
                       lambda h, yb=yb: h.tensor_scalar(out=yb, in0=yb, scalar1=ccol("invf", rows=32), scalar2=None, op0=ALU.mult)]
                for which, add in ((1, 0.0), (0, 0.25)):
                    fns += [
                        lambda h, yc=yc, yb=yb, add=add: h.tensor_scalar(out=yc, in0=yb, scalar1=add, scalar2=None, op0=ALU.add),
                        lambda h, yc=yc, yai=yai: h.tensor_copy(out=yai, in_=yc),
                        lambda h, ya=ya, yai=yai: h.tensor_copy(out=ya, in_=yai),
                        lambda h, yc=yc, ya=ya: h.tensor_tensor(out=yc, in0=yc, in1=ya, op=ALU.subtract),
                        lambda h, yc=yc, ya=ya: h.tensor_scalar(out=ya, in0=yc, scalar1=0.5, scalar2=None, op0=ALU.is_gt),
                        lambda h, yc=yc, ya=ya: h.tensor_tensor(out=yc, in0=yc, in1=ya, op=ALU.subtract),
                        lambda h, yc=yc, ya=ya: h.tensor_scalar(out=ya, in0=yc, scalar1=-0.5, scalar2=None, op0=ALU.is_lt),
                        lambda h, yc=yc, ya=ya: h.tensor_tensor(out=yc, in0=yc, in1=ya, op=ALU.add),
                    ]
                    chain("dve", fns, r=[con_b], w=[yab, ybb, ycb])
                    fns = []
                    P.op("act", lambda h, yc=yc: h.activation(out=yc, in_=yc, func=AF.Sin, scale=2.0 * math.pi * (1.0 - 1e-6)),
                         w=[ycb])
                    if which == 1:
                        P.op("dve", lambda h, yc=yc, tsl=tsl: h.tensor_scalar(
                            out=rot_t[:, 1, tsl], in0=yc, scalar1=ccol("sgn", rows=32), scalar2=None, op0=ALU.mult),
                            r=[ycb, con_b], w=[rot_b])
                    else:
                        P.op("dve", lambda h, yc=yc, tsl=tsl: h.tensor_copy(out=rot_t[:, 0, tsl], in_=yc), r=[ycb], w=[rot_b])

        def apply_rotary(kf, kfb, t):
            tsl = slice(t * TT, (t + 1) * TT)
            pr, prb = pB.next()
            pm = ccol("pm", 0, 32, rows=32)
            P.op("pe", lambda h: h.matmul(pr[0:32, :], lhsT=pm, rhs=kf[0:32, :], start=True, stop=True),
                 r=[kfb, con_b], w=[prb])
            t2, t2b = tmp.next()
            chain("dve", [
                lambda h: h.tensor_tensor(out=t2[0:32, :], in0=pr[0:32, :], in1=rot_t[:, 1, tsl], op=ALU.mult),
                lambda h: h.tensor_tensor(out=kf[0:32, :], in0=kf[0:32, :], in1=rot_t[:, 0, tsl], op=ALU.mult),
                lambda h: h.tensor_tensor(out=kf[0:32, :], in0=kf[0:32, :], in1=t2[0:32, :], op=ALU.add),
            ], r=[prb, rot_b], w=[kfb, t2b])

        def dbg_dump_xT():
            for k in range(KC):
                b_ = Buf()
                P.dma(out_d[:, k * S:(k + 1) * S], xT[:, k, :], r=[xb[k][t] for t in range(NT)], w=[b_])
                out_bufs.append(b_)

        pre = Ring(G32[:, 0:12288].rearrange("p (s n) -> p s n", n=1024), 12, "pre")
        cast_i = [0]

        def pre_cast(dst, src, srcb, dstb):
            eng = ("dve", "act")[cast_i[0] % 2]
            cast_i[0] += 1
            if eng == "dve":
                P.op("dve", lambda h: h.tensor_copy(out=dst, in_=src), r=[srcb], w=[dstb])
            else:
                P.op("act", lambda h: h.copy(out=dst, in_=src), r=[srcb], w=[dstb])

        if dbg is None or dbg[0] in ("x4",):
            for e in range(NE):
                w13v = moe_w13_d[e * D:(e + 1) * D, :].rearrange("(k p) n -> p k n", p=128)
                w2v = moe_w2_d[e * FFE:(e + 1) * FFE, :].rearrange("(c p) n -> p c n", p=128)
                for c in range(MOE_NCH):
                    wb, wbb = wbf.next()
                    for half, co in ((0, c * 128), (1, FFE + c * 128)):
                        st, stb = pre.next()
                        P.dma(st[:, 0:KC * 128].rearrange("p (k n) -> p k n", n=128), w13v[:, :, co:co + 128], w=[stb])
                        pre_cast(wb[:, half * KC * 128:(half + 1) * KC * 128], st[:, 0:KC * 128], stb, wbb)
                    r0 = (e * MOE_NCH + c) * 128
                    P.dma(moe_scr13[r0:r0 + 128, :], wb[:, 0:2 * KC * 128], r=[wbb], w=[scr13_b[e][c]], q="pool")
                for sg in range(MOE_NSEG):
                    for j in range(KC):
                        wb, wbb = wbf.next()
                        st, stb = pre.next()
                        P.dma(st[:, 0:MOE_SEG * 128].rearrange("p (c n) -> p c n", n=128),
                              w2v[:, sg * MOE_SEG:(sg + 1) * MOE_SEG, j * 128:(j + 1) * 128], w=[stb])
                        pre_cast(wb[:, 0:MOE_SEG * 128], st[:, 0:MOE_SEG * 128], stb, wbb)
                        r0 = ((e * MOE_NSEG + sg) * KC + j) * 128
                        P.dma(moe_scr2[r0:r0 + 128, :], wb[:, 0:MOE_SEG * 128], r=[wbb], w=[scr2_b[e][sg][j]], q="pool")

        for s_ in range(nseq):
            pcf = pC_t[:].rearrange("p a n -> p (a n)")
            for tt in range(S // 128):
                st, stb = wst.next()
                P.dma(st[:, 0:D], x_d[s_ * S + tt * 128:s_ * S + (tt + 1) * 128, :], w=[stb])

                def fn_tr(h, st=st):
                    ins = None
                    for k in range(KC):
                        ins = h.transpose(out=pcf[:, k * 128:(k + 1) * 128], in_=st[:, k * 128:(k + 1) * 128], identity=ident_f)
                    return ins
                P.op("pe", fn_tr, r=[stb, con_b], w=[pC_b])
                t = tt // 4
                P.op("act", lambda h, tt=tt: h.copy(out=xT[:, :, tt * 128:(tt + 1) * 128],
                                                    in_=pcf.rearrange("p (k n) -> p k n", n=128)),
                     r=[pC_b], w=[xb[k][t] for k in range(KC)])
            if dbg is not None and dbg[0] == "xT":
                dbg_dump_xT()
                break

            P.op("pool", lambda h: h.memset(small[:, 16:17], 0.0), r=[], w=[Gb, hfb] + pre.b)
            rmsnorm(s_, 0, range(NT))
            UW = S + CW - 1
            uT = G[:, 0:KC * UW].rearrange("p (k n) -> p k n", n=UW)
            P.op("pool", lambda h: h.memset(uT[:, :, 0:CW - 1], 0.0), w=[Gb])
            w1v = conv_w1_d.rearrange("(k p) n -> p k n", p=128)
            for m in range(KC):
                wb, wbb = load_w([(0, w1v[:, :, m * 128:(m + 1) * 128], KC, 128),
                                  (KC * 128, w1v[:, :, D + m * 128:D + (m + 1) * 128], KC, 128)], 2 * KC * 128)
                wa = wb[:, 0:KC * 128].rearrange("p (k n) -> p k n", n=128)
                wg = wb[:, KC * 128:2 * KC * 128].rearrange("p (k n) -> p k n", n=128)
                for t in range(NT):
                    tsl = slice(t * TT, (t + 1) * TT)
                    pa, pab = pA.next()
                    pg, pgb = pA.next()
                    mm_group(pa, [(wa[:, k, :], A[:, k, tsl]) for k in range(KC)], r=[wbb, Ab[t]], w=[pab])
                    mm_group(pg, [(wg[:, k, :], A[:, k, tsl]) for k in range(KC)], r=[wbb, Ab[t]], w=[pgb])
                    sg, sgb = tmp.next()
                    P.op("act", lambda h, sg=sg, pg=pg, m=m: h.activation(out=sg, in_=pg, func=AF.Sigmoid,
                                                                            bias=vcol("cb1", KC + m), scale=1.0),
                         r=[pgb, vecs_b], w=[sgb])
                    P.op("dve", lambda h, sg=sg, pa=pa, m=m, t=t: h.scalar_tensor_tensor(
                        out=uT[:, m, CW - 1 + t * TT:CW - 1 + (t + 1) * TT], in0=pa, scalar=vcol("cb1", m), in1=sg,
                        op0=ALU.add, op1=ALU.mult), r=[pab, sgb, vecs_b], w=[Gb])
            for m in range(KC):
                d1, d1b = wbf.next()
                d2, d2b = wbf.next()
                dv1 = d1[:, 0:16 * 128].rearrange("p (j n) -> p j n", n=128)
                dv2 = d2[:, 0:15 * 128].rearrange("p (j n) -> p j n", n=128)

                def dg(j, dv1=dv1, dv2=dv2):
                    return dv1[:, j, :] if j < 16 else dv2[:, j - 16, :]

                def fn_dg(h, m=m, lo=0, hi=16, dg=dg):
                    ins = None
                    for j in range(lo, hi):
                        ins = h.tensor_scalar(out=dg(j), in0=ident_bf, scalar1=vcol("dww", m * CW + j),
                                              scalar2=None, op0=ALU.mult)
                    return ins
                P.op("pool", fn_dg, r=[cbf_b, vecs_b], w=[d1b])
                P.op("pool", lambda h, f=fn_dg: f(h, lo=16, hi=CW), r=[cbf_b, vecs_b], w=[d2b])
                for t in range(NT):
                    tsl = slice(t * TT, (t + 1) * TT)
                    pz, pzb = pA.next()
                    mm_group(pz, [(dg(j), uT[:, m, t * TT + j:t * TT + j + TT]) for j in range(CW)],
                             r=[d1b, d2b, Gb], w=[pzb])
                    P.op("act", lambda h, pz=pz, m=m, tsl=tsl: h.activation(
                        out=A[:, m, tsl], in_=pz, func=AF.Identity, bias=vcol("dwb", m), scale=1.0),
                        r=[pzb, vecs_b], w=[Ab[t]])
            for t in range(NT):
                tsl = slice(t * TT, (t + 1) * TT)
                zs_t, zsb = wst.next()
                zsq = zs_t.bitcast(BF16)[:, 0:KC * TT].rearrange("p (k n) -> p k n", n=TT)
                P.op("pool", lambda h, zsq=zsq, tsl=tsl: h.tensor_tensor(out=zsq, in0=A[:, :, tsl], in1=A[:, :, tsl], op=ALU.mult),
                     r=[Ab[t]], w=[zsb])
                p1, p1b = pB.next()
                p2, p2b = pB.next()
                mm_group(p1, [(ones_bf, A[:, k, tsl]) for k in range(KC)], r=[Ab[t], cbf_b], w=[p1b])
                mm_group(p2, [(ones_bf, zsq[:, k, :]) for k in range(KC)], r=[zsb, cbf_b], w=[p2b])
                mean, meanb = p1, p1b
                rstd, rstdb = p2, p2b
                msq, msqb = tmp.next()
                P.op("act", lambda h, mean=mean, p1=p1: h.activation(out=mean, in_=p1, func=AF.Copy, scale=1.0 / D), r=[p1b], w=[meanb])
                P.op("act", lambda h, mean=mean, msq=msq: h.activation(out=msq, in_=mean, func=AF.Square), r=[meanb], w=[msqb])
                P.op("dve", lambda h, rstd=rstd, p2=p2, msq=msq: h.scalar_tensor_tensor(
                    out=rstd, in0=p2, scalar=1.0 / D, in1=msq, op0=ALU.mult, op1=ALU.subtract), r=[p2b, msqb], w=[rstdb])
                P.op("act", lambda h, rstd=rstd: h.activation(out=rstd, in_=rstd, func=AF.Sqrt, bias=EPS, scale=1.0), r=[rstdb], w=[rstdb])
                P.op("dve", lambda h, rstd=rstd: h.reciprocal(out=rstd, in_=rstd), r=[rstdb], w=[rstdb])
                for m in range(KC):
                    t1, t1b = tmp.next()
                    P.op("dve", lambda h, t1=t1, m=m, tsl=tsl, mean=mean: h.tensor_tensor(
                        out=t1, in0=A[:, m, tsl], in1=mean, op=ALU.subtract), r=[Ab[t], meanb], w=[t1b])
                    P.op("dve", lambda h, t1=t1, rstd=rstd: h.tensor_tensor(out=t1, in0=t1, in1=rstd, op=ALU.mult),
                         r=[rstdb], w=[t1b])
                    P.op("act", lambda h, t1=t1, m=m, t=t: h.activation(
                        out=uT[:, m, CW - 1 + t * TT:CW - 1 + (t + 1) * TT], in_=t1, func=AF.Silu,
                        bias=vcol("lnb", m), scale=vcol("lng", m)), r=[t1b, vecs_b], w=[Gb])
            w2cv = conv_w2_d.rearrange("(k p) n -> p k n", p=128)
            for j in range(KC):
                wb, wbb = load_w([(0, w2cv[:, :, j * 128:(j + 1) * 128], KC, 128)], KC * 128)
                w2b = wb[:, 0:KC * 128].rearrange("p (k n) -> p k n", n=128)
                for t in range(NT):
                    tsl = slice(t * TT, (t + 1) * TT)
                    py, pyb = pA.next()
                    mm_group(py, [(w2b[:, m, :], uT[:, m, CW - 1 + t * TT:CW - 1 + (t + 1) * TT]) for m in range(KC)],
                             r=[wbb, Gb], w=[pyb])
                    t1, t1b = tmp.next()
                    P.op("act", lambda h, t1=t1, py=py, j=j: h.activation(out=t1, in_=py, func=AF.Identity,
                                                                           bias=vcol("cb2", j), scale=1.0),
                         r=[pyb, vecs_b], w=[t1b])
                    P.op("dve", lambda h, t1=t1, j=j, tsl=tsl, s_=s_: h.scalar_tensor_tensor(
                        out=xT[:, j, tsl], in0=t1, scalar=modv[:, 16 + j, s_:s_ + 1], in1=xT[:, j, tsl],
                        op0=ALU.mult, op1=ALU.add), r=[t1b, modv_b, xb[j][t]], w=[xb[j][t]])
            if dbg is not None and dbg[0] == "x1":
                dbg_dump_xT()
                break
            rmsnorm(s_, 1, range(NT))
            l13, l2 = f32_loaders(ffn_w13_d, ffn_w2_d, FF0)
            ffn(l13, l2, FF0, 11, list(range(NT)), resid_epilogue(40, s_))
            if dbg is not None and dbg[0] == "x2":
                dbg_dump_xT()
                break

            rmsnorm(s_, 2, range(NT))
            rotary_tables(s_)
            if dbg is not None and dbg[0] == "kv0":
                dbg_dump_xT()
                break
            wkvv = w_kv_d.rearrange("(k p) n -> p k n", p=128)
            kbr = Ring(G[:, 0:4 * 1024].rearrange("p (s n) -> p s n", n=1024), 4, "kbr")
            P.op("pool", lambda h: h.memset(small[:, 16:17], 0.0), r=[Gb], w=[Gb] + kbr.b)
            wk_cur = [None]

            def k_stage_a(hd, t):
                if t == 0:
                    wb, wbb = load_w([(0, wkvv[:, :, hd * 128:(hd + 1) * 128], KC, 128)], KC * 128)
                    wk_cur[0] = (wb[:, 0:KC * 128].rearrange("p (k n) -> p k n", n=128), wbb)
                wk, wbb = wk_cur[0]
                tsl = slice(t * TT, (t + 1) * TT)
                pk, pkb = pA.next()
                mm_group(pk, [(wk[:, k, :], A[:, k, tsl]) for k in range(KC)], r=[wbb, Ab[t]], w=[pkb])
                kf, kfb = tmp.next()
                P.op("act", lambda h: h.copy(out=kf, in_=pk), r=[pkb], w=[kfb])
                return (hd, t, kf, kfb)

            def k_stage_b(item):
                hd, t, kf, kfb = item
                tsl = slice(t * TT, (t + 1) * TT)
                apply_rotary(kf, kfb, t)
                kb_t, kbb = kbr.next()
                kb16 = kb_t[:, 0:TT]
                ksq = kb_t[:, TT:2 * TT]
                sm, smb = rt.next()
                P.op("dve", lambda h: h.tensor_reduce(
                    out=sm[:, 0:2], in_=kf.rearrange("p (b n) -> p b n", n=256), axis=AX.X, op=ALU.add),
                    r=[kfb], w=[smb])
                P.op("dve", lambda h: h.tensor_scalar(
                    out=kmb[:, hd, 2 * t:2 * t + 2], in0=sm[:, 0:2], scalar1=1.0 / 256, scalar2=None, op0=ALU.mult),
                    r=[smb], w=[kmb_b])
                P.op("dve", lambda h: h.tensor_copy(out=kb16, in_=kf), r=[kfb], w=[kbb])
                P.op("pool", lambda h: h.tensor_tensor(out=ksq, in0=kf, in1=kf, op=ALU.mult), r=[kfb], w=[kbb])
                P.dma(k_scr[hd * 128:(hd + 1) * 128, tsl], kb16, r=[kbb], w=[kscr_b[hd][t]], q="pool")
                return (hd, t, ksq, kbb)

            def k_stage_c(item):
                hd, t, ksq, kbb = item
                pq, pqb = pA.next()
                P.op("pe", lambda h: h.matmul(pq, lhsT=ones_bf, rhs=ksq, start=True, stop=True),
                     r=[kbb, cbf_b], w=[pqb])
                if t == 0:
                    P.op("dve", lambda h: h.tensor_reduce(out=nhk[:, hd:hd + 1], in_=pq, axis=AX.X, op=ALU.max),
                         r=[pqb], w=[nhk_b])
                else:
                    sm2, sm2b = rt.next()
                    P.op("dve", lambda h: h.tensor_reduce(out=sm2[:, 0:1], in_=pq, axis=AX.X, op=ALU.max),
                         r=[pqb], w=[sm2b])
                    P.op("dve", lambda h: h.tensor_tensor(
                        out=nhk[:, hd:hd + 1], in0=nhk[:, hd:hd + 1], in1=sm2[:, 0:1], op=ALU.max), r=[sm2b], w=[nhk_b])
                if t == NT - 1:
                    P.op("dve", lambda h: h.tensor_scalar(
                        out=nhk[:, hd:hd + 1], in0=nhk[:, hd:hd + 1], scalar1=-0.5, scalar2=None, op0=ALU.mult), w=[nhk_b])

            a_q, b_q = [], []
            for hd in range(NH):
                for t in range(NT):
                    a_q.append(k_stage_a(hd, t))
                    if len(a_q) > 1:
                        b_q.append(k_stage_b(a_q.pop(0)))
                    if len(b_q) > 1:
                        k_stage_c(b_q.pop(0))
            while a_q:
                b_q.append(k_stage_b(a_q.pop(0)))
                if len(b_q) > 1:
                    k_stage_c(b_q.pop(0))
            while b_q:
                k_stage_c(b_q.pop(0))
            if dbg is not None and dbg[0] == "kvK":
                dbg_dump_xT()
                break
            wvb = G[:, 0:KC * D].rearrange("p (k n) -> p k n", n=D)
            P.op("pool", lambda h: h.memset(small[:, 16:17], 0.0), r=kbr.b, w=[Gb] + kbr.b)
            for k in range(KC):
                st, stb = wst.next()
                P.dma(st[:, 0:D], w_kv_d[k * 128:(k + 1) * 128, D:2 * D], w=[stb])
                P.op("pool", lambda h, st=st, k=k: h.tensor_copy(out=wvb[:, k, :], in_=st[:, 0:D]), r=[stb], w=[Gb])
            for tt in range(S // 128):
                t = tt // 4

                def fn_v(h, tt=tt):
                    ins = None
                    for half in range(2):
                        for k in range(KC):
                            ins = h.matmul(pC_t[:, half, :], lhsT=A[:, k, tt * 128:(tt + 1) * 128],
                                           rhs=wvb[:, k, half * TT:(half + 1) * TT], start=(k == 0), stop=(k == KC - 1))
                    return ins
                P.op("pe", fn_v, r=[Ab[t], Gb], w=[pC_b])
                vb_t, vbb = wbf.next()
                P.op("act", lambda h, vb_t=vb_t: h.copy(out=vb_t[:, 0:D], in_=pcf), r=[pC_b], w=[vbb])
                P.dma(v_scr[tt * 128:(tt + 1) * 128, :], vb_t[:, 0:D], r=[vbb], w=[vscr_b[tt]], q="pool")

            if dbg is not None and dbg[0] == "kvV":
                dbg_dump_xT()
                break
            rmsnorm(s_, 3, range(NT))
            OT = G[:, 0:NH * S].rearrange("p (k n) -> p k n", n=S)
            o0 = NH * S
            qT = G[:, o0:o0 + S]
            qsq = G[:, o0 + S:o0 + 2 * S]
            kTh = G[:, o0 + 2 * S:o0 + 3 * S]
            vH = G[:, o0 + 3 * S:o0 + 4 * S].rearrange("p (c n) -> p c n", n=128)
            OTb, qb_, kTb, vHb = Buf("OT"), Buf("q"), Buf("kT"), Buf("vH")
            P.op("pool", lambda h: h.memset(small[:, 16:17], 0.0), r=[Gb], w=[Gb, OTb, qb_, kTb, vHb, pC_b, pC0b, pC1b])
            wqv = w_q_d.rearrange("(k p) n -> p k n", p=128)
            for hd in range(NH):
                wb, wbb = load_w([(0, wqv[:, :, hd * 128:(hd + 1) * 128], KC, 128)], KC * 128)
                wq = wb[:, 0:KC * 128].rearrange("p (k n) -> p k n", n=128)
                P.dma(kTh, k_scr[hd * 128:(hd + 1) * 128, :], r=kscr_b[hd], w=[kTb])
                P.dma(vH, v_scr.rearrange("(c p) n -> p c n", p=128)[:, :, hd * 128:(hd + 1) * 128], r=vscr_b, w=[vHb])
                for t in range(NT):
                    tsl = slice(t * TT, (t + 1) * TT)
                    pk, pkb = pA.next()
                    mm_group(pk, [(wq[:, k, :], A[:, k, tsl]) for k in range(KC)], r=[wbb, Ab[t]], w=[pkb])
                    kf, kfb = tmp.next()
                    P.op("act", lambda h, kf=kf, pk=pk: h.copy(out=kf, in_=pk), r=[pkb], w=[kfb])
                    apply_rotary(kf, kfb, t)
                    P.op("dve", lambda h, kf=kf, tsl=tsl: h.tensor_copy(out=qT[:, tsl], in_=kf), r=[kfb], w=[qb_])
                    P.op("pool", lambda h, kf=kf, tsl=tsl: h.tensor_tensor(out=qsq[:, tsl], in0=kf, in1=kf, op=ALU.mult),
                         r=[kfb], w=[qb_])
                def g_stage1(qc, hd=hd):
                    b = qc // 2
                    qs = slice(qc * 128, (qc + 1) * 128)
                    pg, pgb = pA.next()

                    def fn_g(h):
                        h.matmul(pg[:, 0:8], lhsT=qT[:, qs], rhs=kmb[:, hd, :], start=True, stop=True)
                        return h.matmul(pg[:, 8:9], lhsT=qsq[:, qs], rhs=ones_bf[:, 0:1], start=True, stop=True)
                    P.op("pe", fn_g, r=[qb_, kmb_b, cbf_b], w=[pgb])
                    rtt, rtb = rt.next()
                    o_sm = CL["smask"][0] + b * 8
                    sm, smb = tmp.next()
                    gm = sm[:, 0:8]
                    t8 = sm[:, 8:16]
                    chain("dve", [
                        lambda h: h.tensor_tensor(out=gm, in0=pg[:, 0:8], in1=con[:, o_sm:o_sm + 8], op=ALU.add),
                        lambda h: h.max(out=t8, in_=gm),
                        lambda h: h.tensor_scalar(out=rtt[:, 0:8], in0=gm, scalar1=t8[:, 2:3], scalar2=BIG,
                                                  op0=ALU.is_ge, op1=ALU.mult),
                        lambda h: h.tensor_scalar(out=rtt[:, 8:9], in0=pg[:, 8:9], scalar1=-0.5,
                                                  scalar2=nhk[:, hd:hd + 1], op0=ALU.mult, op1=ALU.add),
                        lambda h: h.memset(rtt[:, 9:10], -BIG),
                    ], r=[pgb, con_b, nhk_b], w=[rtb, smb])
                    return (qs, rtt, rtb)

                def g_stage2(item):
                    qs, rtt, rtb = item
                    ptr, ptrb = pA.next()
                    P.op("pe", lambda h: h.transpose(out=ptr[0:10, 0:128], in_=rtt[:, 0:10], identity=ident_f),
                         r=[rtb, con_b], w=[ptrb])
                    P.op("act", lambda h: h.copy(out=rall[:, qs], in_=ptr[0:10, 0:128]), r=[ptrb], w=[rall_b])

                g_q = []
                for qc in range(S // 128):
                    g_q.append(g_stage1(qc))
                    if len(g_q) > 1:
                        g_stage2(g_q.pop(0))
                while g_q:
                    g_stage2(g_q.pop(0))
                if dbg is not None and dbg[0] == "h0g":
                    break
                acc_sets = [(pB_t[:, 0], pB.b[0], pB_t[:, 1], pB.b[1]), (pC_t[:, 0], pC0b, pC_t[:, 1], pC1b)]

                def s_stage(b, kc):
                    q0 = b * 256
                    n = kc // 2
                    qlo = 128 if kc == 2 * b + 1 else 0
                    var = n if n < b else 8
                    W_ = 256 - qlo
                    pss, pssb = pA.next()

                    def fn_s(h):
                        h.matmul(pss[:, 0:W_], lhsT=kTh[:, kc * 128:(kc + 1) * 128], rhs=qT[:, q0 + qlo:q0 + 256],
                                 start=True, stop=False)
                        return h.matmul(pss[:, 0:W_], lhsT=esel_bf[0:10, var * 128:(var + 1) * 128],
                                        rhs=rall[:, q0 + qlo:q0 + 256], start=False, stop=True)
                    P.op("pe", fn_s, r=[kTb, qb_, rall_b, cbf_b], w=[pssb])
                    ptile, ptb = pt.next()
                    P.op("act", lambda h: h.activation(out=ptile[:, 0:W_], in_=pss[:, 0:W_], func=AF.Exp, scale=ATT_SCALE),
                         r=[pssb], w=[ptb])
                    if kc >= 2 * b:
                        P.op("pool", lambda h: h.tensor_tensor(out=ptile[:, 0:128], in0=ptile[:, 0:128], in1=tri_bf, op=ALU.mult),
                             r=[cbf_b], w=[ptb])
                    return (b, kc, qlo, W_, ptile, ptb)

                def pv_stage(item, hd=hd):
                    b, kc, qlo, W_, ptile, ptb = item
                    nkc = 2 * b + 2
                    q0 = b * 256
                    po, pob, psm, psmb = acc_sets[b % 2]

                    def fn_pv(h):
                        h.matmul(po[:, qlo:256], lhsT=vH[:, kc, :], rhs=ptile[:, 0:W_], start=(kc == 0), stop=(kc == nkc - 1))
                        return h.matmul(psm[:, qlo:256], lhsT=ones_bf, rhs=ptile[:, 0:W_], start=(kc == 0), stop=(kc == nkc - 1))
                    P.op("pe", fn_pv, r=[ptb, vHb, cbf_b], w=[pob, psmb])
                    if kc == nkc - 1:
                        rs, rsb = tmp.next()
                        P.op("dve", lambda h: h.reciprocal(out=rs[:, 0:256], in_=psm[:, 0:256]), r=[psmb], w=[rsb])
                        P.op("dve", lambda h: h.tensor_tensor(
                            out=OT[:, hd, q0:q0 + 256], in0=po[:, 0:256], in1=rs[:, 0:256], op=ALU.mult),
                            r=[rsb, pob], w=[OTb])

                inflight = []
                for b in range(S // 256):
                    for kc in range(2 * b + 2):
                        inflight.append(s_stage(b, kc))
                        if len(inflight) > ATT_DEPTH:
                            pv_stage(inflight.pop(0))
                while inflight:
                    pv_stage(inflight.pop(0))
            if dbg is not None and dbg[0] == "h0g":
                dbg_dump_xT()
                break
            if dbg is not None and dbg[0] == "OT":
                for hd in range(NH):
                    st, stb = wst.next()
                    P.op("act", lambda h, st=st, hd=hd: h.copy(out=st[:, 0:S], in_=OT[:, hd, :]), r=[OTb], w=[stb])
                    b_ = Buf()
                    P.dma(out_d[:, hd * S:(hd + 1) * S], st[:, 0:S], r=[stb], w=[b_])
                    out_bufs.append(b_)
                break
            wov = w_o_d.rearrange("(k p) n -> p k n", p=128)
            for j in range(KC):
                wb, wbb = load_w([(0, wov[:, :, j * 128:(j + 1) * 128], KC, 128)], KC * 128)
                wo = wb[:, 0:KC * 128].rearrange("p (k n) -> p k n", n=128)
                for t in range(NT):
                    tsl = slice(t * TT, (t + 1) * TT)
                    py, pyb = pA.next()
                    mm_group(py, [(wo[:, hd, :], OT[:, hd, tsl]) for hd in range(NH)], r=[wbb, OTb], w=[pyb])
                    P.op("dve", lambda h, py=py, j=j, tsl=tsl, s_=s_: h.scalar_tensor_tensor(
                        out=xT[:, j, tsl], in0=py, scalar=modv[:, 48 + 16 + j, s_:s_ + 1], in1=xT[:, j, tsl],
                        op0=ALU.mult, op1=ALU.add), r=[pyb, modv_b, xb[j][t]], w=[xb[j][t]])
            if dbg is not None and dbg[0] == "x3":
                dbg_dump_xT()
                break

            P.op("pool", lambda h: h.memset(small[:, 16:17], 0.0), r=[], w=[Gb, hfb, OTb, qb_, kTb, vHb, pC_b, pC0b, pC1b])
            for t in range(NT):
                rmsnorm(s_, 4, [t], f32=True)
                for c4 in range(4):
                    qc = t * 4 + c4
                    pl, plb = pB.next()

                    def fn_r(h, pl=pl, c4=c4):
                        for k in range(KC):
                            h.matmul(pl[:, 0:NE], lhsT=hfv[:, k, c4 * 128:(c4 + 1) * 128], rhs=rwf[:, k, :], start=(k == 0), stop=False)
                        return h.matmul(pl[:, 0:NE], lhsT=ones_f[0:1, :], rhs=rbf[0:1, :], start=False, stop=True)
                    P.op("pe", fn_r, r=[hfb, rw_b, ones_fb], w=[plb])
                    sm, smb = tmp.next()
                    lg, t8, nt_, ee, sel, den = sm[:, 0:8], sm[:, 8:16], sm[:, 16:17], sm[:, 24:32], sm[:, 32:40], sm[:, 40:41]
                    chain("dve", [
                        lambda h, lg=lg, pl=pl: h.tensor_copy(out=lg, in_=pl[:, 0:NE]),
                        lambda h, lg=lg, t8=t8: h.max(out=t8, in_=lg),
                        lambda h, t8=t8, nt_=nt_: h.tensor_scalar(out=nt_, in0=t8[:, 0:1], scalar1=-1.0, scalar2=None, op0=ALU.mult),
                        lambda h, lg=lg, t8=t8, sel=sel: h.tensor_scalar(out=sel, in0=lg, scalar1=t8[:, 1:2], scalar2=None, op0=ALU.is_ge),
                    ], r=[plb], w=[smb])
                    P.op("act", lambda h, lg=lg, ee=ee, nt_=nt_: h.activation(out=ee, in_=lg, func=AF.Exp, bias=nt_, scale=1.0),
                         r=[smb], w=[smb])
                    chain("dve", [
                        lambda h, ee=ee, sel=sel: h.tensor_tensor(out=ee, in0=ee, in1=sel, op=ALU.mult),
                        lambda h, ee=ee, den=den: h.tensor_reduce(out=den, in_=ee, axis=AX.X, op=ALU.add),
                        lambda h, den=den: h.reciprocal(out=den, in_=den),
                    ], r=[], w=[smb])
                    P.op("dve", lambda h, ee=ee, den=den, qc=qc: h.tensor_scalar(out=gates[:, qc, :], in0=ee, scalar1=den, scalar2=None,
                                                                               op0=ALU.mult), r=[smb], w=[gates_b])
            gbb = Buf("gb")
            P.op("pool", lambda h: h.memset(small[:, 16:17], 0.0), r=[Gb, hfb], w=[Gb, hfb, gbb])
            for e in range(NE):
                gb = G32[:, 8192:8192 + S]
                for qc in range(S // 128):
                    g1, g1b = tmp.next()
                    P.op("dve", lambda h, g1=g1, qc=qc, e=e: h.tensor_scalar(
                        out=g1[:, 0:128], in0=ones_f[:], scalar1=gates[:, qc, e:e + 1], scalar2=None, op0=ALU.mult),
                        r=[gates_b, ones_fb], w=[g1b])
                    pgt, pgtb = pB.next()
                    P.op("pe", lambda h, pgt=pgt, g1=g1: h.transpose(out=pgt[:, 0:128], in_=g1[:, 0:128], identity=ident_f),
                         r=[g1b, con_b], w=[pgtb])
                    P.op("act", lambda h, pgt=pgt, gb=gb, qc=qc: h.copy(out=gb[:, qc * 128:(qc + 1) * 128], in_=pgt[:, 0:128]),
                         r=[pgtb], w=[gbb])

                def moe_ep(j, t, ti, py, pyb, gb=gb, gbb=gbb, s_=s_):
                    tsl = slice(t * TT, (t + 1) * TT)
                    t1, t1b = tmp.next()
                    P.op("dve", lambda h: h.scalar_tensor_tensor(
                        out=t1, in0=py, scalar=modv[:, 48 + 40 + j, s_:s_ + 1], in1=gb[:, t * TT:(t + 1) * TT],
                        op0=ALU.mult, op1=ALU.mult), r=[pyb, modv_b, gbb], w=[t1b])
                    P.op("pool", lambda h: h.tensor_tensor(out=xT[:, j, tsl], in0=xT[:, j, tsl], in1=t1, op=ALU.add),
                         r=[t1b, xb[j][t]], w=[xb[j][t]])

                def l13(c, e=e):
                    wb, wbb = wbf.next()
                    r0 = (e * MOE_NCH + c) * 128
                    P.dma(wb[:, 0:2 * KC * 128], moe_scr13[r0:r0 + 128, :], r=[scr13_b[e][c]], w=[wbb])
                    return wb, wbb

                def l2(c0, seg, j, e=e):
                    wb, wbb = wbf.next()
                    sg = c0 // MOE_SEG
                    r0 = ((e * MOE_NSEG + sg) * KC + j) * 128
                    P.dma(wb[:, 0:MOE_SEG * 128], moe_scr2[r0:r0 + 128, :], r=[scr2_b[e][sg][j]], w=[wbb])
                    return wb, wbb
                ffn(l13, l2, FFE, MOE_SEG, list(range(NT)), moe_ep)
            if dbg is not None and dbg[0] == "x4":
                dbg_dump_xT()
                break

            P.op("pool", lambda h: h.memset(small[:, 16:17], 0.0), r=[Gb, gbb], w=[Gb, hfb, gbb])
            for t in range(NT):
                rmsnorm(s_, None, [t])
                for c4 in range(4):
                    def fn_to(h, c4=c4):
                        ins = None
                        for k in range(KC):
                            ins = h.transpose(out=pcf[:, k * 128:(k + 1) * 128], in_=hfv[:, k, c4 * 128:(c4 + 1) * 128], identity=ident_f)
                        return ins
                    P.op("pe", fn_to, r=[hfb, con_b], w=[pC_b])
                    st, stb = wst.next()
                    P.op("act", lambda h, st=st: h.copy(out=st[:, 0:D], in_=pcf), r=[pC_b], w=[stb])
                    r0 = s_ * S + t * TT + c4 * 128
                    b_ = Buf()
                    P.dma(out_d[r0:r0 + 128, :], st[:, 0:D], r=[stb], w=[b_], q="pool")
                    out_bufs.append(b_)

        P.op("sp", lambda h: h.nop(), r=out_bufs)
        P.emit()
    return nc


def _prep_shared(inp):
    VL = _vec_layout()
    vecs = np.zeros((128, VL["_n"][0]), np.float32)

    def put(name, v):
        o, n = VL[name]
        vecs[:, o:o + n] = _cols(v)
    put("ada_b0", inp["ada_b"][0]); put("ada_b1", inp["ada_b"][1]); put("kv_ada_b", inp["kv_ada_b"])
    put("n1g0", inp["norm1_g"][0]); put("n1g1", inp["norm1_g"][1])
    put("n2g0", inp["norm2_g"][0]); put("n2g1", inp["norm2_g"][1])
    put("kvg", inp["kv_norm_g"]); put("fing", inp["final_g"])
    put("cb1", inp["conv_b1"][0]); put("dwb", inp["conv_dw_b"][0])
    put("lng", inp["conv_ln_g"][0]); put("lnb", inp["conv_ln_b"][0]); put("cb2", inp["conv_b2"][0])
    o, n = VL["dww"]
    dw = np.asarray(inp["conv_dw_w"][0], np.float32)
    for m in range(KC):
        vecs[:, o + m * CW:o + (m + 1) * CW] = dw[:, m * 128:(m + 1) * 128].T
    c1, c2 = _make_consts()
    f = lambda a, shp: np.ascontiguousarray(np.asarray(a, np.float32).reshape(shp))
    return {
        "vecs": vecs, "consts": c1, "consts2": c2,
        "ada_w": f(inp["ada_w"], (2 * D, 6 * D)), "kv_ada_w": f(inp["kv_ada_w"], (D, 2 * D)),
        "conv_w1": f(inp["conv_w1"], (D, 2 * D)), "conv_w2": f(inp["conv_w2"], (D, D)),
        "w_kv": f(inp["w_kv"], (D, 2 * D)), "w_q": f(inp["w_q"], (D, D)), "w_o": f(inp["w_o"], (D, D)),
        "ffn_w13": f(inp["ffn_w13"], (D, 2 * FF0)), "ffn_w2": f(inp["ffn_w2"], (FF0, D)),
        "router_w": f(inp["router_w"], (D, NE)), "router_b": f(inp["router_b"], (1, NE)),
        "moe_w13": f(inp["moe_w13"], (NE * D, 2 * FFE)), "moe_w2": f(inp["moe_w2"], (NE * FFE, D)),
    }


def _core_inputs(inp, shared, seqs):
    nseq = len(seqs)
    x = np.ascontiguousarray(np.asarray(inp["x"], np.float32)[seqs].reshape(nseq * S, D))
    c = np.asarray(inp["c"], np.float32)[seqs]
    cT = np.ascontiguousarray(c.reshape(nseq, KC, 128).transpose(2, 1, 0).reshape(128, KC * nseq))
    pos = np.ascontiguousarray(np.asarray(inp["positions"], np.int32)[seqs])
    m = dict(shared)
    m.update({"x": x, "cT": cT, "pos": pos})
    return m


def run(inp, n_cores, nseq, dbg=None, seq0=0):
    nc = build(nseq, dbg)
    shared = _prep_shared(inp)
    in_maps = [_core_inputs(inp, shared, list(range(seq0 + c * nseq, seq0 + (c + 1) * nseq))) for c in range(n_cores)]
    res = run_bass_kernel_spmd(nc, in_maps, core_ids=list(range(n_cores)))
    return [r["out"] for r in res.results]


def kernel(**inputs):
    nseq = 32 // N_CORES
    outs = run(inputs, N_CORES, nseq)
    return np.concatenate([o.reshape(nseq, S, D) for o in outs], axis=0).astype(np.float32)
```
